# Optimizing a Trainium2 kernel written in Bass

```python
import math
import jax
import jax.numpy as jnp
from jax import lax
import numpy as np

D_MODEL = 2048
BATCH = 1
SEQ = 16384
DEPTH = 4

HEAD_DIM = 64
D_MIX = D_MODEL
QB = 128
A_HEADS = D_MIX // 4 // (2 * HEAD_DIM)
A_DV = 2 * HEAD_DIM
B_HEADS = D_MIX // 4 // HEAD_DIM
DILATED_PATTERNS = ((128, 1), (512, 4), (2048, 16))
C_HEADS = D_MIX // 2 // HEAD_DIM
C_GROUPS = 2
C_HPG = C_HEADS // C_GROUPS
CMP_LEN = 32
CMP_STRIDE = 16
CMP_HID = 2 * HEAD_DIM
SLC_LEN = 64
N_SELECT = 16
WIN = 512
N_BRANCH = 3
D_FF = 5632
N_EXPERTS = 8
TOP_K = 2
EPS = 1e-6
NEG_INF = -1e30
FORCE_SCORE = 1e6

IN_SIZES = (
    A_HEADS * HEAD_DIM, A_HEADS * HEAD_DIM, A_HEADS * HEAD_DIM, A_HEADS * HEAD_DIM, A_HEADS * A_DV,
    B_HEADS * HEAD_DIM, B_HEADS * HEAD_DIM, B_HEADS * HEAD_DIM,
    C_HEADS * HEAD_DIM,
    C_GROUPS * HEAD_DIM, C_GROUPS * HEAD_DIM, C_GROUPS * HEAD_DIM,
    C_GROUPS * HEAD_DIM, C_GROUPS * HEAD_DIM, C_GROUPS * HEAD_DIM,
    C_HEADS * N_BRANCH,
)
IN_COLS = sum(IN_SIZES)
IN_OFFSETS = tuple(int(c) for c in np.cumsum(IN_SIZES)[:-1])

kernel_name = "hybrid_diff_dilated_nsa_moe_trunk"


def rmsnorm(x, g):
    xf = x.astype(jnp.float32)
    y = xf * lax.rsqrt(jnp.mean(xf * xf, axis=-1, keepdims=True) + EPS)
    return (y * g.astype(jnp.float32)).astype(x.dtype)


def alibi_slopes(n):
    return jnp.exp2(-8.0 * jnp.arange(1, n + 1, dtype=jnp.float32) / n)


def split_heads(t, n):
    return t.reshape(t.shape[:-1] + (n, t.shape[-1] // n))


def masked_softmax(s, valid):
    s = jnp.where(valid, s, NEG_INF)
    m = jnp.max(s, axis=-1, keepdims=True)
    e = jnp.where(valid, jnp.exp(s - m), 0.0)
    den = jnp.maximum(jnp.sum(e, axis=-1, keepdims=True), 1e-30)
    return e / den, (m + jnp.log(den))[..., 0]


def diff_attention(q1, q2, k1, k2, v, lam, lam_init, g_sub):
    B, S, H, dk = q1.shape
    dv = v.shape[-1]
    nb = S // QB
    slopes = alibi_slopes(H)
    qs = jnp.stack([q1, q2], axis=2).reshape(B, nb, QB, 2, H, dk).transpose(1, 0, 2, 3, 4, 5)
    ks = jnp.stack([k1, k2], axis=2)
    kpos = jnp.arange(S)

    def block(args):
        qblk, i = args
        qpos = i * QB + jnp.arange(QB)
        dist = qpos[:, None] - kpos[None, :]
        s = jnp.einsum("bqmhd,bkmhd->bmhqk", qblk, ks, preferred_element_type=jnp.float32) * dk ** -0.5
        s = s - slopes[:, None, None] * dist
        p, _ = masked_softmax(s, dist >= 0)
        a = p[:, 0] - lam * p[:, 1]
        return jnp.einsum("bhqk,bkhd->bqhd", a.astype(v.dtype), v)

    o = lax.map(block, (qs, jnp.arange(nb)))
    o = o.transpose(1, 0, 2, 3, 4).reshape(B, S, H, dv)
    o = rmsnorm(o, g_sub) * (1.0 - lam_init)
    return o.reshape(B, S, H * dv)


def banded_attention(q, k, v, nw, step, slopes):
    N, L, H, dh = q.shape
    nb = -(-L // QB)
    extra = nb * QB - L
    qb = jnp.pad(q, ((0, 0), (0, extra), (0, 0), (0, 0))).reshape(N, nb, QB, H, dh)
    pad_kv = ((0, 0), (nw, extra), (0, 0), (0, 0))
    idx = jnp.arange(nb)[:, None] * QB + jnp.arange(QB + nw)[None, :]
    kb = jnp.pad(k, pad_kv)[:, idx]
    vb = jnp.pad(v, pad_kv)[:, idx]
    rel = (jnp.arange(QB)[:, None] + nw) - jnp.arange(QB + nw)[None, :]
    kabs = idx - nw
    valid = (rel >= 0) & (rel <= nw) & (kabs >= 0)[:, None, :]
    s = jnp.einsum("nbqhd,nbkhd->nbhqk", qb, kb, preferred_element_type=jnp.float32) * dh ** -0.5
    s = s - slopes[:, None, None] * (rel * step)
    p, lse = masked_softmax(s, valid[None, :, None])
    o = jnp.einsum("nbhqk,nbkhd->nbqhd", p.astype(v.dtype), vb)
    o = o.reshape(N, nb * QB, H, dh)[:, :L]
    lse = lse.transpose(0, 1, 3, 2).reshape(N, nb * QB, H)[:, :L]
    return o, lse


def to_residue(t, dil):
    B, S = t.shape[:2]
    rest = t.shape[2:]
    perm = (0, 2, 1) + tuple(range(3, 3 + len(rest)))
    return t.reshape((B, S // dil, dil) + rest).transpose(perm).reshape((B * dil, S // dil) + rest)


def from_residue(t, B, dil):
    L = t.shape[1]
    rest = t.shape[2:]
    perm = (0, 2, 1) + tuple(range(3, 3 + len(rest)))
    return t.reshape((B, dil, L) + rest).transpose(perm).reshape((B, L * dil) + rest)


def dilated_attention(q, k, v):
    B, S, H, dh = q.shape
    slopes = alibi_slopes(H)
    outs, lses = [], []
    for window, dil in DILATED_PATTERNS:
        o, lse = banded_attention(to_residue(q, dil), to_residue(k, dil), to_residue(v, dil),
                                  window // dil, dil, slopes)
        outs.append(from_residue(o, B, dil))
        lses.append(from_residue(lse, B, dil))
    w = jax.nn.softmax(jnp.stack(lses, axis=0), axis=0)
    o = jnp.einsum("pbsh,pbshd->bshd", w.astype(q.dtype), jnp.stack(outs, axis=0))
    return o.reshape(B, S, H * dh)


def nsa_compress(t, pos, w1, w2):
    B, S, G, dk = t.shape
    c = t.reshape(B, S // CMP_STRIDE, CMP_STRIDE, G, dk)
    blk = jnp.concatenate([c[:, :-1], c[:, 1:]], axis=2) + pos[:, None, :]
    blk = blk.transpose(0, 1, 3, 2, 4).reshape(B, S // CMP_STRIDE - 1, G, CMP_LEN * dk)
    return jax.nn.silu(blk @ w1) @ w2


def cmp_to_slc(imp, n_slc):
    r = SLC_LEN // CMP_STRIDE
    lead = CMP_LEN // CMP_STRIDE - 1
    padded = jnp.pad(imp, [(0, 0)] * (imp.ndim - 1) + [(lead, r)])
    return sum(padded[..., o:o + r * n_slc:r] for o in range(r + lead))


def nsa_attention(q, k_cmp, v_cmp, k_slc, v_slc, k_win, v_win, gate_logits):
    B, S, H, dk = q.shape
    G = k_slc.shape[2]
    Hg = H // G
    nb = S // QB
    n_slc = S // SLC_LEN
    n_sel = min(N_SELECT, n_slc)
    n_cmp = k_cmp.shape[1]
    scale = dk ** -0.5
    slopes = alibi_slopes(H).reshape(G, Hg)
    cmp_end = jnp.arange(n_cmp) * CMP_STRIDE + CMP_LEN - 1
    k_blocks = k_slc.reshape(B, n_slc, SLC_LEN, G, dk).transpose(0, 3, 1, 2, 4)
    v_blocks = v_slc.reshape(B, n_slc, SLC_LEN, G, dk).transpose(0, 3, 1, 2, 4)
    kw = jnp.pad(k_win, ((0, 0), (WIN, 0), (0, 0), (0, 0)))
    vw = jnp.pad(v_win, ((0, 0), (WIN, 0), (0, 0), (0, 0)))
    gather = jax.vmap(jax.vmap(lambda blocks, ix: blocks[ix]))
    qs = q.reshape(B, nb, QB, G, Hg, dk).transpose(1, 0, 2, 3, 4, 5)
    gs = jax.nn.sigmoid(gate_logits.astype(jnp.float32)).reshape(B, nb, QB, G, Hg, N_BRANCH)
    gs = gs.transpose(1, 0, 2, 3, 4, 5)
    blk_ids = jnp.arange(n_slc)

    def block(args):
        qblk, gblk, i = args
        t = i * QB + jnp.arange(QB)
        d_c = t[:, None] - cmp_end[None, :]
        s = jnp.einsum("bqghd,bngd->bghqn", qblk, k_cmp, preferred_element_type=jnp.float32) * scale
        s = s - slopes[:, :, None, None] * d_c
        p_c, _ = masked_softmax(s, d_c >= 0)
        o_c = jnp.einsum("bghqn,bngd->bqghd", p_c.astype(q.dtype), v_cmp)
        imp = cmp_to_slc(jnp.sum(p_c, axis=2), n_slc)
        tb = t // SLC_LEN
        forced = (blk_ids[None, :] == 0) | (blk_ids[None, :] == tb[:, None]) | (blk_ids[None, :] == tb[:, None] - 1)
        allowed = blk_ids[None, :] * SLC_LEN <= t[:, None]
        score = jnp.where(allowed, jnp.where(forced, FORCE_SCORE, imp), -1.0)
        _, sel = lax.top_k(score, n_sel)
        ks = gather(k_blocks, sel).reshape(B, G, QB, n_sel * SLC_LEN, dk)
        vs = gather(v_blocks, sel).reshape(B, G, QB, n_sel * SLC_LEN, dk)
        pos = (sel[..., None] * SLC_LEN + jnp.arange(SLC_LEN)).reshape(B, G, QB, n_sel * SLC_LEN)
        d_s = t[None, None, :, None] - pos
        s = jnp.einsum("bqghd,bgqkd->bghqk", qblk, ks, preferred_element_type=jnp.float32) * scale
        s = s - slopes[None, :, :, None, None] * d_s[:, :, None]
        p_s, _ = masked_softmax(s, (d_s >= 0)[:, :, None])
        o_s = jnp.einsum("bghqk,bgqkd->bqghd", p_s.astype(q.dtype), vs)
        kwin = lax.dynamic_slice_in_dim(kw, i * QB, QB + WIN, axis=1)
        vwin = lax.dynamic_slice_in_dim(vw, i * QB, QB + WIN, axis=1)
        kpos = i * QB - WIN + jnp.arange(QB + WIN)
        d_w = t[:, None] - kpos[None, :]
        valid_w = (d_w >= 0) & (d_w < WIN) & (kpos >= 0)[None, :]
        s = jnp.einsum("bqghd,bkgd->bghqk", qblk, kwin, preferred_element_type=jnp.float32) * scale
        s = s - slopes[:, :, None, None] * d_w
        p_w, _ = masked_softmax(s, valid_w)
        o_w = jnp.einsum("bghqk,bkgd->bqghd", p_w.astype(q.dtype), vwin)
        o = gblk[..., 0:1] * o_c + gblk[..., 1:2] * o_s + gblk[..., 2:3] * o_w
        return o.astype(q.dtype)

    o = lax.map(block, (qs, gs, jnp.arange(nb)))
    return o.transpose(1, 0, 2, 3, 4, 5).reshape(B, S, H * dk)


def swiglu(h, wg, wu, wd):
    return (jax.nn.silu(h @ wg) * (h @ wu)) @ wd


def moe_swiglu(h, w_router, b_router, wg, wu, wd):
    logits = jnp.einsum("bsd,de->bse", h, w_router, preferred_element_type=jnp.float32)
    logits = logits + b_router.astype(jnp.float32)
    top_logit, top_idx = lax.top_k(logits, TOP_K)
    top_w = jax.nn.softmax(top_logit, axis=-1)
    combine = jnp.einsum("bsk,bske->bse", top_w, jax.nn.one_hot(top_idx, N_EXPERTS, dtype=jnp.float32))
    out = jnp.zeros(h.shape, jnp.float32)
    for e in range(N_EXPERTS):
        out = out + combine[..., e:e + 1] * swiglu(h, wg[e], wu[e], wd[e])
    return out.astype(h.dtype)


def setup_inputs(seed: int = 0) -> dict:
    key = jax.random.key(seed)
    keys = iter(jax.random.split(key, 32))

    def nrm(shape, scale):
        return scale * jax.random.normal(next(keys), shape, jnp.float32)

    n_dense = (DEPTH + 1) // 2
    n_moe = DEPTH // 2
    out_scale = (2 * DEPTH) ** -0.5
    return {
        "x": nrm((BATCH, SEQ, D_MODEL), 1.0),
        "ln_attn": 1.0 + nrm((DEPTH, D_MODEL), 0.02),
        "w_in": nrm((DEPTH, D_MODEL, IN_COLS), D_MODEL ** -0.5),
        "w_out": nrm((DEPTH, D_MIX, D_MODEL), D_MIX ** -0.5 * out_scale),
        "lam_q1": nrm((DEPTH, HEAD_DIM), 0.1),
        "lam_k1": nrm((DEPTH, HEAD_DIM), 0.1),
        "lam_q2": nrm((DEPTH, HEAD_DIM), 0.1),
        "lam_k2": nrm((DEPTH, HEAD_DIM), 0.1),
        "subln": 1.0 + nrm((DEPTH, A_DV), 0.02),
        "cmp_pos_k": nrm((DEPTH, CMP_LEN, HEAD_DIM), 0.1),
        "cmp_w1_k": nrm((DEPTH, CMP_LEN * HEAD_DIM, CMP_HID), (CMP_LEN * HEAD_DIM) ** -0.5),
        "cmp_w2_k": nrm((DEPTH, CMP_HID, HEAD_DIM), CMP_HID ** -0.5),
        "cmp_pos_v": nrm((DEPTH, CMP_LEN, HEAD_DIM), 0.1),
        "cmp_w1_v": nrm((DEPTH, CMP_LEN * HEAD_DIM, CMP_HID), (CMP_LEN * HEAD_DIM) ** -0.5),
        "cmp_w2_v": nrm((DEPTH, CMP_HID, HEAD_DIM), CMP_HID ** -0.5),
        "ln_ffn": 1.0 + nrm((DEPTH, D_MODEL), 0.02),
        "ffn_w_gate": nrm((n_dense, D_MODEL, D_FF), D_MODEL ** -0.5),
        "ffn_w_up": nrm((n_dense, D_MODEL, D_FF), D_MODEL ** -0.5),
        "ffn_w_down": nrm((n_dense, D_FF, D_MODEL), D_FF ** -0.5 * out_scale),
        "router_w": nrm((n_moe, D_MODEL, N_EXPERTS), D_MODEL ** -0.5),
        "router_b": nrm((n_moe, N_EXPERTS), 0.01),
        "exp_w_gate": nrm((n_moe, N_EXPERTS, D_MODEL, D_FF), D_MODEL ** -0.5),
        "exp_w_up": nrm((n_moe, N_EXPERTS, D_MODEL, D_FF), D_MODEL ** -0.5),
        "exp_w_down": nrm((n_moe, N_EXPERTS, D_FF, D_MODEL), D_FF ** -0.5 * out_scale),
        "ln_final": 1.0 + nrm((D_MODEL,), 0.02),
    }


def reference(x, ln_attn, w_in, w_out, lam_q1, lam_k1, lam_q2, lam_k2, subln,
              cmp_pos_k, cmp_w1_k, cmp_w2_k, cmp_pos_v, cmp_w1_v, cmp_w2_v,
              ln_ffn, ffn_w_gate, ffn_w_up, ffn_w_down,
              router_w, router_b, exp_w_gate, exp_w_up, exp_w_down, ln_final):
    B, S, _ = x.shape
    for l in range(DEPTH):
        h = rmsnorm(x, ln_attn[l])
        proj = jnp.einsum("bsd,dc->bsc", h, w_in[l])
        (aq1, aq2, ak1, ak2, av, bq, bk, bv, cq,
         ckc, cvc, cks, cvs, ckw, cvw, cg) = jnp.split(proj, IN_OFFSETS, axis=-1)
        lam_init = 0.8 - 0.6 * math.exp(-0.3 * l)
        lam = (jnp.exp(jnp.sum(lam_q1[l] * lam_k1[l]).astype(jnp.float32))
               - jnp.exp(jnp.sum(lam_q2[l] * lam_k2[l]).astype(jnp.float32)) + lam_init)
        oa = diff_attention(split_heads(aq1, A_HEADS), split_heads(aq2, A_HEADS),
                            split_heads(ak1, A_HEADS), split_heads(ak2, A_HEADS),
                            split_heads(av, A_HEADS), lam, lam_init, subln[l])
        ob = dilated_attention(split_heads(bq, B_HEADS), split_heads(bk, B_HEADS), split_heads(bv, B_HEADS))
        kc = nsa_compress(split_heads(ckc, C_GROUPS), cmp_pos_k[l], cmp_w1_k[l], cmp_w2_k[l])
        vc = nsa_compress(split_heads(cvc, C_GROUPS), cmp_pos_v[l], cmp_w1_v[l], cmp_w2_v[l])
        oc = nsa_attention(split_heads(cq, C_HEADS), kc, vc,
                           split_heads(cks, C_GROUPS), split_heads(cvs, C_GROUPS),
                           split_heads(ckw, C_GROUPS), split_heads(cvw, C_GROUPS),
                           cg.reshape(B, S, C_HEADS, N_BRANCH))
        mix = jnp.concatenate([oa, ob, oc], axis=-1)
        x = x + jnp.einsum("bsc,cd->bsd", mix, w_out[l])
        h = rmsnorm(x, ln_ffn[l])
        j = l // 2
        if l % 2 == 0:
            x = x + swiglu(h, ffn_w_gate[j], ffn_w_up[j], ffn_w_down[j])
        else:
            x = x + moe_swiglu(h, router_w[j], router_b[j], exp_w_gate[j], exp_w_up[j], exp_w_down[j])
    return rmsnorm(x, ln_final)
```

```python
import math
from contextlib import ExitStack
import numpy as np
import ml_dtypes
import concourse.bass as bass
import concourse.mybir as mybir
from concourse.bass_utils import run_bass_kernel_spmd

F32 = mybir.dt.float32
BF16 = mybir.dt.bfloat16
AF = mybir.ActivationFunctionType
ALU = mybir.AluOpType
AX = mybir.AxisListType
NPBF = ml_dtypes.bfloat16

NCORES = 8
D = 2048
S = 16384
NBLK = 128
DFF = 5632
EPS = 1e-6
INCOLS = 4912


def gblock(c, lb):
    return 16 * (lb // 2) + (c if lb % 2 == 0 else 15 - c)


class Buf:
    __slots__ = ("name", "w", "r", "sem", "semval")

    def __init__(self, name=""):
        self.name = name
        self.w = None
        self.r = {}
        self.sem = None
        self.semval = 0


class Sched:
    ENGS = ("pe", "act", "dve", "pool", "sp")

    def __init__(self, nc):
        self.nc = nc
        self.ops = {e: [] for e in self.ENGS}
        self.cnt = {e: 0 for e in self.ENGS}
        self.seen = {e: {} for e in self.ENGS}
        self.nd = 0
        self.outbufs = []

    def _deps(self, eng, reads, writes):
        best = {}
        for b in reads:
            if b.w is not None:
                k, v = b.w
                if best.get(k, 0) < v:
                    best[k] = v
        for b in writes:
            if b.w is not None:
                k, v = b.w
                if best.get(k, 0) < v:
                    best[k] = v
            for k, v in b.r.items():
                if best.get(k, 0) < v:
                    best[k] = v
        seen = self.seen[eng]
        waits = []
        for k, v in best.items():
            if k == eng:
                if eng == "pe" or self.cnt[eng] - v >= 8:
                    continue
            if seen.get(k, 0) >= v:
                continue
            seen[k] = v
            waits.append((k, v))
        return waits

    def _mark(self, tok, reads, writes):
        k, v = tok
        for b in reads:
            if b.r.get(k, 0) < v:
                b.r[k] = v
        for b in writes:
            b.w = tok
            b.r = {}

    def op(self, eng, fn, reads=(), writes=()):
        waits = self._deps(eng, reads, writes)
        self.cnt[eng] += 1
        tok = (eng, self.cnt[eng])
        self.ops[eng].append((waits, fn, tok))
        self._mark(tok, reads, writes)
        return tok

    def dma(self, queue, pairs, reads=(), writes=(), sembuf=None):
        waits = self._deps(queue, reads, writes)
        if sembuf.sem is None:
            sembuf.sem = self.nd
            self.nd += 1
        key = ("d", sembuf.sem)
        for i, (o, a) in enumerate(pairs):
            sembuf.semval += 16
            self.ops[queue].append((waits if i == 0 else [],
                                    (lambda e, o=o, a=a: e.dma_start(out=o, in_=a)),
                                    (key, sembuf.semval)))
        tok = (key, sembuf.semval)
        self._mark(tok, reads, writes)
        return tok

    def realias(self, new_bufs, old_bufs):
        acc = {}
        for b in old_bufs:
            if b.w is not None:
                k, v = b.w
                acc[k] = max(acc.get(k, 0), v)
            for k, v in b.r.items():
                acc[k] = max(acc.get(k, 0), v)
        for b in new_bufs:
            b.w = None
            b.r = dict(acc)

    def finish(self, outbufs):
        self.op("sp", lambda e: "nop", reads=outbufs)

    def emit(self, st):
        nc = self.nc
        esem = {e: st.enter_context(nc.semaphore("s_" + e)) for e in self.ENGS}
        dsem = [st.enter_context(nc.semaphore("d%d" % i)) for i in range(self.nd)]
        block = st.enter_context(nc.Block())

        def mk(name):
            def body(e):
                for waits, fn, tok in self.ops[name]:
                    for k, v in waits:
                        e.wait_ge(esem[k] if isinstance(k, str) else dsem[k[1]], v)
                    ins = fn(e)
                    if isinstance(ins, str):
                        continue
                    assert ins is not None, ("builder returned None", name, tok)
                    if isinstance(tok[0], str):
                        ins.then_inc(esem[tok[0]], 1)
                    else:
                        ins.then_inc(dsem[tok[0][1]], 16)
            return body

        block.tensor(mk("pe"))
        block.scalar(mk("act"))
        block.vector(mk("dve"))
        block.gpsimd(mk("pool"))
        block.sync(mk("sp"))


class Ctx:
    def __init__(self, nc, st):
        self.nc = nc
        self.st = st
        self.n = 0

    def sb(self, shape, dt, name=None):
        self.n += 1
        return self.st.enter_context(self.nc.sbuf_tensor(name or ("t%d" % self.n), list(shape), dt))

    def ps(self, shape, dt, name=None):
        self.n += 1
        return self.st.enter_context(self.nc.psum_tensor(name or ("p%d" % self.n), list(shape), dt))

    def din(self, name, shape, dt):
        return self.nc.dram_tensor(name, list(shape), dt, kind="ExternalInput").ap()

    def dout(self, name, shape, dt):
        return self.nc.dram_tensor(name, list(shape), dt, kind="ExternalOutput").ap()


def rr(i, lst):
    return lst[i % len(lst)]


def emit_rmsnorm_T(s, cx, src_ap, src_buf, gbc, gbuf, hT, hTbuf, tcol, ident, identb, wk, scale_dim=D):
    i = wk["i"]
    wk["i"] += 1
    junk, junkb = rr(i, wk["junk"])
    ss, ssb = rr(i, wk["ss"])
    hb, hbb = rr(i, wk["hb"])
    s.op("act", lambda e: e.activation(out=junk[:], in_=src_ap, func=AF.Square, accum_out=ss[:, 0:1]),
         reads=[src_buf], writes=[junkb, ssb])
    s.op("dve", lambda e: e.tensor_scalar(out=ss[:, 32:33], in0=ss[:, 0:1], scalar1=1.0 / scale_dim, scalar2=EPS,
                                          op0=ALU.mult, op1=ALU.add), reads=[ssb], writes=[ssb])
    s.op("act", lambda e: e.activation(out=ss[:, 64:65], in_=ss[:, 32:33], func=AF.Sqrt), reads=[ssb], writes=[ssb])
    s.op("dve", lambda e: e.reciprocal(out=ss[:, 96:97], in_=ss[:, 64:65]), reads=[ssb], writes=[ssb])
    s.op("dve", lambda e: e.scalar_tensor_tensor(out=hb[:], in0=src_ap, scalar=ss[:, 96:97], in1=gbc[:],
                                                 op0=ALU.mult, op1=ALU.mult),
         reads=[src_buf, ssb, gbuf], writes=[hbb])
    for half in range(2):
        ptr, ptrb = rr(2 * i + half, wk["ptr"])
        for k in range(8):
            dc = half * 8 + k
            s.op("pe", lambda e, dc=dc, k=k, ptr=ptr: e.transpose(out=ptr[:, k * 128:(k + 1) * 128],
                                                                 in_=hb[:, dc * 128:(dc + 1) * 128], identity=ident[:]),
                 reads=[hbb, identb], writes=[ptrb])
        dst = hT[:, half * 8:half * 8 + 8, tcol:tcol + 128]
        srcp = ptr[:].rearrange("p (a b) -> p a b", a=8)
        if (i + half) % 2 == 0:
            s.op("act", lambda e, dst=dst, srcp=srcp: e.activation(out=dst, in_=srcp, func=AF.Copy),
                 reads=[ptrb], writes=[hTbuf])
        else:
            s.op("dve", lambda e, dst=dst, srcp=srcp: e.tensor_copy(out=dst, in_=srcp),
                 reads=[ptrb], writes=[hTbuf])


def mk_norm_work(cx):
    wk = {"i": 0}
    wk["junk"] = [(cx.sb([128, D], BF16), Buf("junk"))]
    wk["ss"] = [(cx.sb([128, 128], F32), Buf("ss")) for _ in range(2)]
    wk["hb"] = [(cx.sb([128, D], BF16), Buf("hb")) for _ in range(2)]
    wk["ptr"] = [(cx.ps([128, 1024], BF16), Buf("ptr")) for _ in range(2)]
    return wk


def k1_chunks():
    ch = []
    for h in range(4):
        ch.append(([(0 + 64 * h, 64), (256 + 64 * h, 64)], 0.125))
    for h in range(4):
        ch.append(([(512 + 64 * h, 64), (768 + 64 * h, 64)], 1.0))
    for c in range(4):
        ch.append(([(1536 + 128 * c, 128)], 0.125))
    for c in range(4):
        ch.append(([(2048 + 128 * c, 128)], 1.0))
    for j in range(8):
        ch.append(([(3072 + 64 * j, 64), (3072 + 64 * (8 + j), 64)], 0.125))
    for c0 in (4096, 4224, 4352, 4608):
        ch.append(([(c0, 128)], 1.0))
    return ch


NFCH = 28
VCOLS = 1296
TOKCOLS = [(1024, 512), (2560, 512), (4480, 128), (4736, 128), (4864, 48)]


def build_k1(ntiles=16):
    nc = bass.Bass("TRN2", target_bir_lowering=False)
    NT = ntiles * 128
    with ExitStack() as st:
        cx = Ctx(nc, st)
        s = Sched(nc)
        x = cx.din("x", [NT, D], F32)
        g = cx.din("g", [1, D], F32)
        w = cx.din("w", [D, INCOLS], F32)
        idn = cx.din("ident", [128, 128], BF16)
        featT = cx.dout("featT", [NFCH, 128, NT], BF16)
        tokM = cx.dout("tokM", [NT, VCOLS], BF16)
        gates = cx.dout("gates", [NT, 48], F32)
        b_featT, b_tokM, b_gates = Buf("featT"), Buf("tokM"), Buf("gates")

        ident = cx.sb([128, 128], BF16); identb = Buf("ident")
        gbc = cx.sb([128, D], F32); gbuf = Buf("gbc")
        xt = [(cx.sb([128, D], F32), Buf("xt")) for _ in range(2)]
        hT = cx.sb([128, 16, NT], BF16); hTb = Buf("hT")
        wtok = cx.sb([128, 16, 1328], BF16); wtokb = Buf("wtok")
        wst = [(cx.sb([128, 16, 128], BF16), Buf("wst")) for _ in range(2)]
        fst = [(cx.sb([128, NT], BF16), Buf("fst")) for _ in range(2)]
        tst = [(cx.sb([128, VCOLS], BF16), Buf("tst")) for _ in range(2)]
        gst = [(cx.sb([128, 48], F32), Buf("gst")) for _ in range(2)]
        wk = mk_norm_work(cx)
        pf = [(cx.ps([128, 512], F32), Buf("pf")) for _ in range(3)]
        pt = [(cx.ps([128, 512], F32), Buf("pt")) for _ in range(3)]

        s.dma("sp", [(ident[:], idn)], writes=[identb], sembuf=identb)
        s.dma("sp", [(gbc[:], g.partition_broadcast(128))], writes=[gbuf], sembuf=gbuf)
        for (tt, tb_) in tst:
            s.op("pool", lambda e, tt=tt: e.memset(tt[:], 1.0), writes=[tb_])
        wv = w.rearrange("(dc p) c -> p dc c", p=128)
        off = 0
        pairs = []
        for c0, ln in TOKCOLS:
            pairs.append((wtok[:, :, off:off + ln], wv[:, :, c0:c0 + ln]))
            off += ln
        s.dma("pool", pairs, writes=[wtokb], sembuf=wtokb)
        for t in range(ntiles):
            xtt, xtb = rr(t, xt)
            s.dma("sp", [(xtt[:], x[t * 128:(t + 1) * 128, :])], writes=[xtb], sembuf=xtb)
            emit_rmsnorm_T(s, cx, xtt[:], xtb, gbc, gbuf, hT, hTb, t * 128, ident, identb, wk)
        ntg = (NT + 511) // 512
        ev = 0
        for c, (segs, scale) in enumerate(k1_chunks()):
            wt, wtb = rr(c, wst)
            pairs = []
            off = 0
            for c0, ln in segs:
                pairs.append((wt[:, :, off:off + ln], wv[:, :, c0:c0 + ln]))
                off += ln
            s.dma("pool", pairs, writes=[wtb], sembuf=wtb)
            fs, fsb = rr(c, fst)
            for tg in range(ntg):
                n = min(512, NT - tg * 512)
                p, pb = rr(ev, pf)
                for dc in range(16):
                    s.op("pe", lambda e, p=p, wt=wt, dc=dc, tg=tg, n=n: e.matmul(
                        p[:, 0:n], wt[:, dc, :], hT[:, dc, tg * 512:tg * 512 + n], start=(dc == 0), stop=(dc == 15)),
                        reads=[wtb, hTb], writes=[pb])
                if ev % 2 == 0:
                    s.op("act", lambda e, p=p, fs=fs, tg=tg, n=n, scale=scale: e.activation(
                        out=fs[:, tg * 512:tg * 512 + n], in_=p[:, 0:n], func=AF.Copy, scale=scale),
                        reads=[pb], writes=[fsb])
                else:
                    s.op("dve", lambda e, p=p, fs=fs, tg=tg, n=n, scale=scale: e.tensor_scalar(
                        out=fs[:, tg * 512:tg * 512 + n], in0=p[:, 0:n], scalar1=scale, scalar2=None, op0=ALU.mult),
                        reads=[pb], writes=[fsb])
                ev += 1
            s.dma("sp", [(featT[c], fs[:])], reads=[fsb], writes=[b_featT], sembuf=fsb)
        for t in range(ntiles):
            ts_, tsb = rr(t, tst)
            gs, gsb = rr(t, gst)
            for gi, (c0, n) in enumerate([(0, 512), (512, 512), (1024, 304)]):
                p, pb = rr(ev, pt)
                for dc in range(16):
                    s.op("pe", lambda e, p=p, dc=dc, t=t, c0=c0, n=n: e.matmul(
                        p[:, 0:n], hT[:, dc, t * 128:(t + 1) * 128], wtok[:, dc, c0:c0 + n],
                        start=(dc == 0), stop=(dc == 15)), reads=[wtokb, hTb], writes=[pb])
                if gi == 0:
                    mv = [(ts_[:, 0:516].rearrange("p (h d) -> p h d", h=4)[:, :, 0:128], p[:, 0:512].rearrange("p (h d) -> p h d", h=4))]
                elif gi == 1:
                    mv = [(ts_[:, 516:1036].rearrange("p (h d) -> p h d", h=8)[:, :, 0:64], p[:, 0:512].rearrange("p (h d) -> p h d", h=8))]
                else:
                    mv = [(ts_[:, 1036:1166].rearrange("p (h d) -> p h d", h=2)[:, :, 0:64], p[:, 0:128].rearrange("p (h d) -> p h d", h=2)),
                          (ts_[:, 1166:1296].rearrange("p (h d) -> p h d", h=2)[:, :, 0:64], p[:, 128:256].rearrange("p (h d) -> p h d", h=2)),
                          (gs[:], p[:, 256:304])]
                for (o_, i_) in mv:
                    wb_ = gsb if o_ is mv[-1][0] and gi == 2 else tsb
                    if t % 2 == 0:
                        s.op("act", lambda e, o_=o_, i_=i_: e.activation(out=o_, in_=i_, func=AF.Copy), reads=[pb], writes=[wb_])
                    else:
                        s.op("dve", lambda e, o_=o_, i_=i_: e.tensor_copy(out=o_, in_=i_), reads=[pb], writes=[wb_])
                ev += 1
            s.dma("sp", [(tokM[t * 128:(t + 1) * 128, :], ts_[:])], reads=[tsb], writes=[b_tokM], sembuf=tsb)
            s.dma("sp", [(gates[t * 128:(t + 1) * 128, :], gs[:])], reads=[gsb], writes=[b_gates], sembuf=gsb)
        s.finish([b_featT, b_tokM, b_gates])
        s.emit(st)
    return nc


def ident_np():
    return np.eye(128, dtype=np.float32).astype(NPBF)


def build_k3(nexp, nhalf=2, nslot=DFF // 256):
    nc = bass.Bass("TRN2", target_bir_lowering=False)
    moe = nexp > 1
    NT = nhalf * 1024
    with ExitStack() as st:
        cx = Ctx(nc, st)
        s = Sched(nc)
        x = cx.din("x", [NT, D], F32)
        mix = cx.din("mix", [NT, D], BF16)
        wo = cx.din("wo", [D, D], F32)
        gf = cx.din("gf", [1, D], F32)
        idn = cx.din("ident", [128, 128], BF16)
        wg = cx.din("wg", [nexp, D, DFF], F32)
        wu = cx.din("wu", [nexp, D, DFF], F32)
        wd = cx.din("wd", [nexp, DFF, D], F32)
        xo = cx.dout("xo", [NT, D], F32)
        b_xo = Buf("xo")
        outs = [b_xo]
        if moe:
            wr = cx.din("wr", [D, 8], F32)
            rb = cx.din("rb", [1, 8], F32)
            gfin = cx.din("gfin", [1, D], F32)
            xf = cx.dout("xf", [NT, D], F32)
            b_xf = Buf("xf")
            outs.append(b_xf)

        ident = cx.sb([128, 128], BF16); identb = Buf("ident")
        gbc = cx.sb([128, D], F32); gbuf = Buf("gbc")
        yacc = cx.sb([128, 8, D], F32); yb = [Buf("y%d" % i) for i in range(8)]
        hT = cx.sb([128, 16, 1024], BF16); hTb = Buf("hT")
        wk = {"i": 0}
        wk["junk"] = [(cx.sb([128, D], BF16), Buf("junk"))]
        wk["ss"] = [(cx.sb([128, 128], F32), Buf("ss")) for _ in range(2)]
        wk["hb"] = [(cx.sb([128, D], BF16), Buf("hb"))]
        wk["ptr"] = [(cx.ps([128, 1024], BF16), Buf("ptr")) for _ in range(2)]
        ring = cx.sb([128, 2, 12288], BF16)
        slot_b = [Buf("slot0"), Buf("slot1")]
        f1_b = [Buf("mixt0"), Buf("mixt1"), Buf("xpc0"), Buf("xpc1"), Buf("wos0"), Buf("wos1")]

        def slot_views(k):
            base = ring[:, k, :]
            wgv = base[:, 0:4096].rearrange("p (a b) -> p a b", a=16)
            wuv = base[:, 4096:8192].rearrange("p (a b) -> p a b", a=16)
            wdv = base[:, 8192:12288].rearrange("p (a b) -> p a b", a=2)
            return wgv, wuv, wdv
        mixt = [ring[:, 0, 0:2048], ring[:, 0, 2048:4096]]
        xpc = [ring[:, 0, 4096:5120].bitcast(F32), ring[:, 0, 5120:6144].bitcast(F32)]
        wos = [ring[:, 0, 6144:12288], ring[:, 1, 0:6144]]
        OCG = 384
        ocgs = [(c0, min(OCG, D - c0)) for c0 in range(0, D, OCG)]
        actT = [(cx.sb([128, 1024], BF16), Buf("actT")) for _ in range(2)]
        sg = [(cx.sb([128, 1024], BF16), Buf("sg")) for _ in range(2)]
        pG = [(cx.ps([128, 1024], F32), Buf("pG"))]
        pU = [(cx.ps([128, 1024], F32), Buf("pU"))]
        pD = [(cx.ps([128, 512], F32), Buf("pD")) for _ in range(2)]
        allps = [pG[0], pU[0]] + pD
        if moe:
            wrb = cx.sb([128, 16, 8], BF16); wrbb = Buf("wrb")
            rbbc = cx.sb([128, 8], F32); rbb = Buf("rbbc")
            call = cx.sb([128, 8, 8], F32); cb = [Buf("c%d" % i) for i in range(8)]
            rt = [(cx.sb([128, 192], F32), Buf("rt")) for _ in range(2)]
            s.dma("pool", [(wrb[:], wr.rearrange("(dc p) c -> p dc c", p=128))], writes=[wrbb], sembuf=wrbb)
            s.dma("sp", [(rbbc[:], rb.partition_broadcast(128))], writes=[rbb], sembuf=rbb)
        s.dma("sp", [(ident[:], idn)], writes=[identb], sembuf=identb)
        wov = wo.rearrange("(dc p) c -> p dc c", p=128)
        ev = 0
        for hf in range(nhalf):
            r0 = hf * 1024
            s.dma("sp", [(gbc[:], gf.partition_broadcast(128))], writes=[gbuf], sembuf=gbuf)
            s.realias(f1_b, slot_b)
            for t in range(8):
                mt, mtb = mixt[t % 2], f1_b[t % 2]
                s.dma("sp", [(mt, mix[r0 + t * 128:r0 + (t + 1) * 128, :])], writes=[mtb], sembuf=mtb)
                for half in range(2):
                    ptr, ptrb = rr(2 * t + half, wk["ptr"])
                    for k in range(8):
                        dc = half * 8 + k
                        s.op("pe", lambda e, dc=dc, k=k, ptr=ptr, mt=mt: e.transpose(
                            out=ptr[:, k * 128:(k + 1) * 128], in_=mt[:, dc * 128:(dc + 1) * 128], identity=ident[:]),
                            reads=[mtb, identb], writes=[ptrb])
                    dst = hT[:, half * 8:half * 8 + 8, t * 128:(t + 1) * 128]
                    srcp = ptr[:].rearrange("p (a b) -> p a b", a=8)
                    if half == 0:
                        s.op("act", lambda e, dst=dst, srcp=srcp: e.activation(out=dst, in_=srcp, func=AF.Copy),
                             reads=[ptrb], writes=[hTb])
                    else:
                        s.op("dve", lambda e, dst=dst, srcp=srcp: e.tensor_copy(out=dst, in_=srcp),
                             reads=[ptrb], writes=[hTb])
            for ci, (c0, cw) in enumerate(ocgs):
                wv_, wb_ = wos[ci % 2], f1_b[4 + ci % 2]
                wv3 = wv_.rearrange("p (a b) -> p a b", a=16)
                s.dma("pool", [(wv3[:, :, 0:cw], wov[:, :, c0:c0 + cw])], writes=[wb_], sembuf=wb_)
                for t in range(8):
                    xp, xpb = xpc[ev % 2], f1_b[2 + ev % 2]
                    s.dma("sp", [(xp[:, 0:cw], x[r0 + t * 128:r0 + (t + 1) * 128, c0:c0 + cw])], writes=[xpb], sembuf=xpb)
                    p, pb = rr(ev, allps)
                    for dc in range(16):
                        s.op("pe", lambda e, p=p, dc=dc, t=t, wv3=wv3, cw=cw: e.matmul(
                            p[:, 0:cw], hT[:, dc, t * 128:(t + 1) * 128], wv3[:, dc, 0:cw],
                            start=(dc == 0), stop=(dc == 15)), reads=[hTb, wb_], writes=[pb])
                    s.op("dve", lambda e, p=p, t=t, c0=c0, cw=cw, xp=xp: e.tensor_tensor(
                        out=yacc[:, t, c0:c0 + cw], in0=p[:, 0:cw], in1=xp[:, 0:cw], op=ALU.add),
                        reads=[pb, xpb], writes=[yb[t]])
                    ev += 1
            for t in range(8):
                emit_rmsnorm_T(s, cx, yacc[:, t, :], yb[t], gbc, gbuf, hT, hTb, t * 128, ident, identb, wk)
            if moe:
                for t in range(8):
                    p, pb = rr(t, pD)
                    r_, rb_ = rr(t, rt)
                    for dc in range(16):
                        s.op("pe", lambda e, p=p, dc=dc, t=t: e.matmul(
                            p[:, 0:8], hT[:, dc, t * 128:(t + 1) * 128], wrb[:, dc, :],
                            start=(dc == 0), stop=(dc == 15)), reads=[hTb, wrbb], writes=[pb])
                    s.op("dve", lambda e, p=p, r_=r_: e.tensor_tensor(out=r_[:, 0:8], in0=p[:, 0:8], in1=rbbc[:], op=ALU.add),
                         reads=[pb, rbb], writes=[rb_])
                    s.op("dve", lambda e, r_=r_: e.max(out=r_[:, 8:16], in_=r_[:, 0:8]), reads=[rb_], writes=[rb_])
                    s.op("dve", lambda e, r_=r_: e.tensor_tensor(out=r_[:, 16:17], in0=r_[:, 9:10], in1=r_[:, 8:9], op=ALU.subtract),
                         reads=[rb_], writes=[rb_])
                    s.op("act", lambda e, r_=r_: e.activation(out=r_[:, 128:129], in_=r_[:, 16:17], func=AF.Exp),
                         reads=[rb_], writes=[rb_])
                    s.op("dve", lambda e, r_=r_: e.tensor_scalar(out=r_[:, 18:19], in0=r_[:, 128:129], scalar1=1.0, scalar2=None, op0=ALU.add),
                         reads=[rb_], writes=[rb_])
                    s.op("dve", lambda e, r_=r_: e.reciprocal(out=r_[:, 19:20], in_=r_[:, 18:19]), reads=[rb_], writes=[rb_])
                    s.op("dve", lambda e, r_=r_: e.tensor_tensor(out=r_[:, 20:21], in0=r_[:, 128:129], in1=r_[:, 19:20], op=ALU.mult),
                         reads=[rb_], writes=[rb_])
                    s.op("dve", lambda e, r_=r_: e.tensor_scalar(out=r_[:, 24:32], in0=r_[:, 0:8], scalar1=r_[:, 8:9], scalar2=r_[:, 19:20],
                                                                op0=ALU.is_equal, op1=ALU.mult), reads=[rb_], writes=[rb_])
                    s.op("dve", lambda e, r_=r_: e.tensor_scalar(out=r_[:, 32:40], in0=r_[:, 0:8], scalar1=r_[:, 9:10], scalar2=r_[:, 20:21],
                                                                op0=ALU.is_equal, op1=ALU.mult), reads=[rb_], writes=[rb_])
                    s.op("dve", lambda e, r_=r_, t=t: e.tensor_tensor(out=call[:, t, :], in0=r_[:, 24:32], in1=r_[:, 32:40], op=ALU.add),
                         reads=[rb_], writes=[cb[t]])
            s.realias(slot_b, f1_b)
            G, Gb = pG[0]
            U, Ub = pU[0]
            pending = None
            units = [(t, dg) for t in range(8) for dg in range(4)]

            def down(pend, lo, hi):
                nonlocal ev
                (wdv, slb, acts, e_) = pend
                for (t, dg) in units[lo:hi]:
                    p, pb = rr(ev, pD)
                    for a in range(2):
                        at, atb = acts[a]
                        s.op("pe", lambda e, p=p, at=at, wdv=wdv, a=a, t=t, dg=dg: e.matmul(
                            p[:, :], at[:, t * 128:(t + 1) * 128], wdv[:, a, dg * 512:(dg + 1) * 512],
                            start=(a == 0), stop=(a == 1)), reads=[atb, slb], writes=[pb])
                    if moe:
                        s.op("dve", lambda e, p=p, t=t, dg=dg, e_=e_: e.scalar_tensor_tensor(
                            out=yacc[:, t, dg * 512:(dg + 1) * 512], in0=p[:, :], scalar=call[:, t, e_:e_ + 1],
                            in1=yacc[:, t, dg * 512:(dg + 1) * 512], op0=ALU.mult, op1=ALU.add),
                            reads=[pb, cb[t], yb[t]], writes=[yb[t]])
                    else:
                        s.op("dve", lambda e, p=p, t=t, dg=dg: e.tensor_tensor(
                            out=yacc[:, t, dg * 512:(dg + 1) * 512], in0=p[:, :],
                            in1=yacc[:, t, dg * 512:(dg + 1) * 512], op=ALU.add),
                            reads=[pb, yb[t]], writes=[yb[t]])
                    ev += 1

            k = 0
            for e_ in range(nexp):
                wgv_d = wg[e_].rearrange("(dc p) f -> p dc f", p=128)
                wuv_d = wu[e_].rearrange("(dc p) f -> p dc f", p=128)
                for sl in range(nslot):
                    f0 = sl * 256
                    wgv, wuv, wdv = slot_views(k % 2)
                    slb = slot_b[k % 2]
                    s.dma("pool", [(wgv, wgv_d[:, :, f0:f0 + 256]), (wuv, wuv_d[:, :, f0:f0 + 256]),
                                   (wdv, wd[e_, f0:f0 + 256, :].rearrange("(a p) d -> p a d", p=128))],
                          writes=[slb], sembuf=slb)
                    for a in range(2):
                        for (P_, Pb_, wv_) in ((G, Gb, wgv), (U, Ub, wuv)):
                            for tg in range(2):
                                for dc in range(16):
                                    s.op("pe", lambda e, P_=P_, wv_=wv_, dc=dc, tg=tg, a=a: e.matmul(
                                        P_[:, tg * 512:(tg + 1) * 512], wv_[:, dc, a * 128:(a + 1) * 128],
                                        hT[:, dc, tg * 512:(tg + 1) * 512], start=(dc == 0), stop=(dc == 15)),
                                        reads=[slb, hTb], writes=[Pb_])
                        sg_, sgb = sg[a]
                        at, atb = actT[a]
                        if pending is not None and a == 0:
                            down(pending, 0, 32)
                            pending = None
                        s.op("act", lambda e, sg_=sg_: e.activation(out=sg_[:], in_=G[:, :], func=AF.Silu),
                             reads=[Gb], writes=[sgb])
                        s.op("dve", lambda e, sg_=sg_, at=at: e.tensor_tensor(out=at[:], in0=sg_[:], in1=U[:, :], op=ALU.mult),
                             reads=[sgb, Ub], writes=[atb])
                    pending = (wdv, slb, [actT[0], actT[1]], e_)
                    k += 1
            down(pending, 0, 32)
            pending = None
            if moe:
                s.dma("sp", [(gbc[:], gfin.partition_broadcast(128))], writes=[gbuf], sembuf=gbuf)
            for t in range(8):
                s.dma("sp", [(xo[r0 + t * 128:r0 + (t + 1) * 128, :], yacc[:, t, :])], reads=[yb[t]], writes=[b_xo], sembuf=yb[t])
                if moe:
                    ss, ssb = rr(t, wk["ss"])
                    junk, junkb = wk["junk"][0]
                    s.op("act", lambda e, t=t, ss=ss, junk=junk: e.activation(out=junk[:], in_=yacc[:, t, :], func=AF.Square, accum_out=ss[:, 0:1]),
                         reads=[yb[t]], writes=[junkb, ssb])
                    s.op("dve", lambda e, ss=ss: e.tensor_scalar(out=ss[:, 32:33], in0=ss[:, 0:1], scalar1=1.0 / D, scalar2=EPS,
                                                                 op0=ALU.mult, op1=ALU.add), reads=[ssb], writes=[ssb])
                    s.op("act", lambda e, ss=ss: e.activation(out=ss[:, 64:65], in_=ss[:, 32:33], func=AF.Sqrt), reads=[ssb], writes=[ssb])
                    s.op("dve", lambda e, ss=ss: e.reciprocal(out=ss[:, 96:97], in_=ss[:, 64:65]), reads=[ssb], writes=[ssb])
                    s.op("dve", lambda e, t=t, ss=ss: e.scalar_tensor_tensor(out=yacc[:, t, :], in0=yacc[:, t, :], scalar=ss[:, 96:97], in1=gbc[:],
                                                                           op0=ALU.mult, op1=ALU.mult),
                         reads=[ssb, gbuf, yb[t]], writes=[yb[t]])
                    s.dma("sp", [(xf[r0 + t * 128:r0 + (t + 1) * 128, :], yacc[:, t, :])], reads=[yb[t]], writes=[b_xf], sembuf=yb[t])
        s.finish(outs)
        s.emit(st)
    return nc


A_SLOPES = [2.0 ** (-8.0 * (i + 1) / 4) for i in range(4)]
B_SLOPES = [2.0 ** (-8.0 * (i + 1) / 8) for i in range(8)]
C_SLOPES = [2.0 ** (-8.0 * (i + 1) / 16) for i in range(16)]
NEGB = -30000.0
CB_ID, CB_CAUS, CB_MA, CB_MW, CB_MB, CB_N = 0, 128, 256, 256 + 2048, 256 + 2048 + 3072, 256 + 2048 + 3072 + 6144
CF_TD, CF_TA, CF_TF = 0, 2176, 2176 + 1088
CF_BC = CF_TF + 1088
CF_BW = CF_BC + 16 * 2 * 136
CF_BA = CF_BW + 16 * 2 * 12
CF_BB = CF_BA + 4 * 2 * 136
CF_N = CF_BB + 8 * 2 * 24


def slot_info(lb):
    par, m = lb % 2, lb // 2
    imin = 16 * m + (0 if par == 0 else 8)
    return par, imin, imin + 7


def k2_consts(c):
    k = np.arange(128)[:, None]
    q = np.arange(128)[None, :]
    caus = (k <= q).astype(np.float32)
    anti = (k > q).astype(np.float32)
    cb = np.zeros((128, CB_N), np.float32)
    cb[:, CB_ID:CB_ID + 128] = np.eye(128)
    cb[:, CB_CAUS:CB_CAUS + 128] = caus
    cf = np.zeros((128, CF_N), np.float32)
    qi = np.arange(128)[:, None]
    for par in range(2):
        e = (7 - c) if par == 0 else c
        for d in range(8):
            dl = d - e
            mk = np.zeros((128, 128)) if dl < 0 else (caus if dl == 0 else np.ones((128, 128)))
            cb[:, CB_MA + (par * 8 + d) * 128:CB_MA + (par * 8 + d + 1) * 128] = mk
        for d in range(12):
            dl = d - e
            mk = caus if dl == 0 else (np.ones((128, 128)) if 1 <= dl <= 3 else (anti if dl == 4 else np.zeros((128, 128))))
            cb[:, CB_MW + (par * 12 + d) * 128:CB_MW + (par * 12 + d + 1) * 128] = mk
        for d in range(24):
            dl = d - e
            if 0 <= dl <= 16:
                dist = 128 * dl + q - k
                ok = (dist >= 0)
                mk = (ok & (dist <= 128)).astype(np.float32) + (ok & (dist % 4 == 0) & (dist <= 512)) + (ok & (dist % 16 == 0) & (dist <= 2048))
            else:
                mk = np.zeros((128, 128))
            cb[:, CB_MB + (par * 24 + d) * 128:CB_MB + (par * 24 + d + 1) * 128] = mk
        z = np.arange(1088)[None, :]
        mm = z - 1016 + 8 * e
        dd = qi - 16 * mm - 31
        cf[:, CF_TD + par * 1088:CF_TD + (par + 1) * 1088] = np.where(dd >= 0, -dd, -1e9)
        z = np.arange(544)[None, :]
        y = z - 256 + 2 * e
        allow = (y <= 0) | ((y == 1) & (qi >= 64))
        forced = (y == 0) | ((y == -1) & (qi < 64)) | ((y == 1) & (qi >= 64))
        cf[:, CF_TA + par * 544:CF_TA + (par + 1) * 544] = allow
        cf[:, CF_TF + par * 544:CF_TF + (par + 1) * 544] = np.where(forced, 1e6, -1.0)
        kk = np.arange(128)[:, None]
        for (base, slopes, nd, lo, hi) in ((CF_BA, A_SLOPES, 136, 0, 127), (CF_BC, C_SLOPES, 136, 0, 127),
                                           (CF_BW, C_SLOPES, 12, 0, 4), (CF_BB, B_SLOPES, 24, 0, 16)):
            for h, sl in enumerate(slopes):
                d = np.arange(nd)[None, :]
                dl = d - e
                val = sl * (kk - 64 - 128 * dl)
                val = np.where((dl >= lo) & (dl <= hi), val, NEGB)
                o = base + (h * 2 + par) * nd
                cf[:, o:o + nd] = val
    return cb.astype(NPBF), cf.astype(np.float32)


def build_k2(slots=tuple(range(16)), do_a=True, do_b=True, do_c=True):
    nc = bass.Bass("TRN2", target_bir_lowering=False)
    NS = 16
    NQ = NS * 128
    with ExitStack() as st:
        cx = Ctx(nc, st)
        s = Sched(nc)
        nkc, nvc, nqc = (4, 516, 4) if do_a else ((4, 520, 4) if do_b else (2, 260, 8))
        kT = cx.din("kT", [nkc, 128, S], BF16)
        vv = cx.din("vv", [S, nvc], BF16)
        qT = cx.din("qT", [nqc, 128, NQ], BF16)
        if do_c:
            gat = cx.din("gat", [NQ, 48], F32)
        cbd = cx.din("cb", [128, CB_N], BF16)
        cfd = cx.din("cf", [128, CF_N], F32)
        if do_a:
            lamv = cx.din("lamv", [1, 256], F32)
            lcst = cx.din("lcst", [1, 2], F32)
            subln = cx.din("subln", [1, 128], F32)
        ocols = 512 if (do_a or do_b) else 1024
        mixo = cx.dout("mixo", [NQ, ocols], BF16)
        b_mix = Buf("mixo")

        cbn = CB_MW if do_a else (CB_N if do_b else CB_MB)
        cf0, cf1 = (CF_BA, CF_BB) if do_a else ((CF_BB, CF_N) if do_b else (0, CF_BA))
        cb = cx.sb([128, cbn], BF16); cbb = Buf("cb")
        cf_t = cx.sb([128, cf1 - cf0], F32); cfb = Buf("cf")
        s.dma("sp", [(cb[:], cbd[:, 0:cbn])], writes=[cbb], sembuf=cbb)
        s.dma("sp", [(cf_t[:], cfd[:, cf0:cf1])], writes=[cfb], sembuf=cfb)

        class _CF:
            def __getitem__(self, idx):
                p_, c_ = idx
                return cf_t[p_, slice(c_.start - cf0, c_.stop - cf0)]
        cf = _CF()
        ident = cb[:, CB_ID:CB_ID + 128]
        arenaK = cx.sb([128, S], BF16); aKb = Buf("arenaK")
        arenaV = cx.sb([128, 128 * 130], BF16); aVb = Buf("arenaV")
        sc2 = [(cx.ps([128, 1024], F32), Buf("sc")) for _ in range(2)]
        accC = cx.ps([128, 1024], F32)
        accb = [Buf("acc0"), Buf("acc1")]
        mb = [(cx.ps([128, 512], F32), Buf("mb")) for _ in range(2)]
        PT = [(cx.sb([128, 8, 128], BF16), Buf("PT")) for _ in range(3)]
        md = [(cx.sb([128, 128], BF16), Buf("md")) for _ in range(2)]
        qb_ = [(cx.sb([128, 8, 128], BF16), Buf("q")) for _ in range(2)]
        stg = [(cx.sb([128, 1024], BF16), Buf("stg")) for _ in range(2)]
        sm = [(cx.sb([128, 512], F32), Buf("sm")) for _ in range(2)]
        nev = {"sc": 0, "pt": 0, "md": 0, "q": 0, "stg": 0, "sm": 0, "mb": 0}

        def nxt(key, lst):
            i = nev[key]
            nev[key] += 1
            return lst[i % len(lst)]

        def exp_mask_av(sc, scb, nh, ncols, bias_of, mask_ap, mask_buf, av_of, acc_of, accbuf, first, per_bank=4, vbuf=None, sc_off=lambda h: h * 128):
            pt, ptb = nxt("pt", PT)
            for h in range(nh):
                s.op("act", lambda e, h=h, pt=pt, sc=sc: e.activation(
                    out=pt[:, h, 0:ncols] if ncols == 128 else pt[:, 2 * h:2 * h + 2, :],
                    in_=sc[:, sc_off(h):sc_off(h) + 128] if ncols == 128 else sc[:, :].rearrange("p (a b) -> p a b", a=2)[:, :, 0:128],
                    func=AF.Exp, bias=bias_of(h)), reads=[scb, cfb], writes=[ptb])
            nt = nh * (ncols // 128)
            if mask_ap is not None:
                s.op("dve", lambda e, pt=pt: e.tensor_tensor(
                    out=pt[:, 0:nt, :], in0=pt[:, 0:nt, :], in1=mask_ap.unsqueeze(1).broadcast_to([128, nt, 128]), op=ALU.mult),
                    reads=[mask_buf, ptb], writes=[ptb])
            for j in range(nt):
                s.op("pe", lambda e, j=j, pt=pt: e.matmul(acc_of(j), pt[:, j, :], av_of(j), start=(first and j % per_bank == 0), stop=True,
                                                         skip_group_check=True), reads=[ptb, vbuf or aVb], writes=accbuf)

        if do_a:
            lam_t = cx.sb([128, 512], F32); lamb = Buf("lam")
            gsub = cx.sb([128, 128], F32); gsb_ = Buf("gsub")
            s.dma("sp", [(lam_t[:, 0:256], lamv.partition_broadcast(128)), (lam_t[:, 256:258], lcst.partition_broadcast(128))],
                  writes=[lamb], sembuf=lamb)
            s.dma("sp", [(gsub[:], subln.partition_broadcast(128))], writes=[gsb_], sembuf=gsb_)
            s.op("dve", lambda e: e.tensor_tensor(out=lam_t[:, 0:64], in0=lam_t[:, 0:64], in1=lam_t[:, 64:128], op=ALU.mult), reads=[lamb], writes=[lamb])
            s.op("dve", lambda e: e.tensor_tensor(out=lam_t[:, 128:192], in0=lam_t[:, 128:192], in1=lam_t[:, 192:256], op=ALU.mult), reads=[lamb], writes=[lamb])
            s.op("dve", lambda e: e.tensor_reduce(out=lam_t[:, 288:289], in_=lam_t[:, 0:64], axis=AX.X, op=ALU.add), reads=[lamb], writes=[lamb])
            s.op("dve", lambda e: e.tensor_reduce(out=lam_t[:, 289:290], in_=lam_t[:, 128:192], axis=AX.X, op=ALU.add), reads=[lamb], writes=[lamb])
            s.op("act", lambda e: e.activation(out=lam_t[:, 320:322], in_=lam_t[:, 288:290], func=AF.Exp), reads=[lamb], writes=[lamb])
            s.op("dve", lambda e: e.tensor_tensor(out=lam_t[:, 352:353], in0=lam_t[:, 320:321], in1=lam_t[:, 321:322], op=ALU.subtract), reads=[lamb], writes=[lamb])
            s.op("dve", lambda e: e.tensor_tensor(out=lam_t[:, 353:354], in0=lam_t[:, 352:353], in1=lam_t[:, 256:257], op=ALU.add), reads=[lamb], writes=[lamb])
            s.op("dve", lambda e: e.tensor_scalar(out=lam_t[:, 354:355], in0=lam_t[:, 353:354], scalar1=-1.0, scalar2=None, op0=ALU.mult), reads=[lamb], writes=[lamb])
            s.op("dve", lambda e: e.tensor_scalar(out=gsub[:], in0=gsub[:], scalar1=lam_t[:, 257:258], scalar2=None, op0=ALU.mult), reads=[lamb, gsb_], writes=[gsb_])
            NLAM = 354
            for h in range(4):
                s.dma("sp", [(arenaK[:, :], kT[h])], writes=[aKb], sembuf=aKb)
                avv = arenaV[:, 0:128 * 129].rearrange("p (b d) -> p b d", b=128)
                s.dma("sp", [(avv[:, b0:b0 + 8, :], vv[b0 * 128:(b0 + 8) * 128, h * 129:(h + 1) * 129].rearrange("(b p) d -> p b d", p=128))
                             for b0 in range(0, 128, 8)], writes=[aVb], sembuf=aVb)
                for lb in slots:
                    par, imin, imax = slot_info(lb)
                    qs, qsb = nxt("q", qb_)
                    s.dma("sp", [(qs[:, 0, :], qT[h, :, lb * 128:(lb + 1) * 128])], writes=[qsb], sembuf=qsb)
                    ab = accb[lb % 2]
                    acc = accC[:, (lb % 2) * 512:(lb % 2) * 512 + 512]
                    for kb in range(imax + 1):
                        d = imax - kb
                        sc, scb = nxt("sc", sc2)
                        for m_ in range(2):
                            s.op("pe", lambda e, sc=sc, m_=m_, kb=kb, qs=qs: e.matmul(
                                sc[:, m_ * 512:m_ * 512 + 128], arenaK[64 * m_:64 * m_ + 64, kb * 128:(kb + 1) * 128],
                                qs[64 * m_:64 * m_ + 64, 0, :], start=True, stop=True), reads=[aKb, qsb], writes=[scb])
                        bo = CF_BA + (h * 2 + par) * 136 + d
                        mk = cb[:, CB_MA + (par * 8 + d) * 128:CB_MA + (par * 8 + d + 1) * 128] if d < 8 else None
                        exp_mask_av(sc, scb, 1, 256, lambda hh, bo=bo: cf[:, bo:bo + 1], mk, cbb,
                                    lambda j, kb=kb, avv=avv: avv[:, kb, :], lambda j, acc=acc: acc[:, j * 256:j * 256 + 129], [ab], kb == 0)
                    sm_, smb = nxt("sm", sm)
                    og, ogb = nxt("stg", stg)
                    s.op("dve", lambda e, sm_=sm_, acc=acc: e.reciprocal(out=sm_[:, 0:1], in_=acc[:, 128:129]), reads=[ab], writes=[smb])
                    s.op("dve", lambda e, sm_=sm_, acc=acc: e.reciprocal(out=sm_[:, 1:2], in_=acc[:, 256 + 128:256 + 129]), reads=[ab], writes=[smb])
                    s.op("dve", lambda e, sm_=sm_: e.tensor_tensor(out=sm_[:, 2:3], in0=sm_[:, 1:2], in1=lam_t[:, NLAM:NLAM + 1], op=ALU.mult), reads=[lamb, smb], writes=[smb])
                    s.op("dve", lambda e, sm_=sm_, acc=acc: e.tensor_scalar(out=sm_[:, 128:256], in0=acc[:, 0:128], scalar1=sm_[:, 0:1], scalar2=None, op0=ALU.mult), reads=[ab, smb], writes=[smb])
                    s.op("dve", lambda e, sm_=sm_, acc=acc: e.scalar_tensor_tensor(out=sm_[:, 128:256], in0=acc[:, 256:384], scalar=sm_[:, 2:3], in1=sm_[:, 128:256], op0=ALU.mult, op1=ALU.add), reads=[ab, smb], writes=[smb])
                    s.op("dve", lambda e, sm_=sm_: e.tensor_tensor(out=sm_[:, 256:384], in0=sm_[:, 128:256], in1=sm_[:, 128:256], op=ALU.mult), reads=[smb], writes=[smb])
                    s.op("dve", lambda e, sm_=sm_: e.tensor_reduce(out=sm_[:, 16:17], in_=sm_[:, 256:384], axis=AX.X, op=ALU.add), reads=[smb], writes=[smb])
                    s.op("dve", lambda e, sm_=sm_: e.tensor_scalar(out=sm_[:, 17:18], in0=sm_[:, 16:17], scalar1=1.0 / 128, scalar2=EPS, op0=ALU.mult, op1=ALU.add), reads=[smb], writes=[smb])
                    s.op("act", lambda e, sm_=sm_: e.activation(out=sm_[:, 64:65], in_=sm_[:, 17:18], func=AF.Sqrt), reads=[smb], writes=[smb])
                    s.op("dve", lambda e, sm_=sm_: e.reciprocal(out=sm_[:, 18:19], in_=sm_[:, 64:65]), reads=[smb], writes=[smb])
                    s.op("dve", lambda e, sm_=sm_, og=og: e.scalar_tensor_tensor(out=og[:, 0:128], in0=sm_[:, 128:256], scalar=sm_[:, 18:19], in1=gsub[:], op0=ALU.mult, op1=ALU.mult), reads=[smb, gsb_], writes=[ogb])
                    s.dma("sp", [(mixo[lb * 128:(lb + 1) * 128, h * 128:(h + 1) * 128], og[:, 0:128])], reads=[ogb], writes=[b_mix], sembuf=ogb)

        if do_b:
            for lb in slots:
                par, imin, imax = slot_info(lb)
                lo = max(0, imin - 16)
                nkb = imax - lo + 1
                kv = arenaK[:, 0:4 * nkb * 128].rearrange("p (c n) -> p c n", c=4)
                vvw = arenaV[:, 0:nkb * 520].rearrange("p (b d) -> p b d", b=nkb)
                s.dma("sp", [(kv[:, cch, :], kT[cch, :, lo * 128:(imax + 1) * 128]) for cch in range(4)], writes=[aKb], sembuf=aKb)
                s.dma("sp", [(vvw, vv[lo * 128:(imax + 1) * 128, 0:520].rearrange("(b p) d -> p b d", p=128))], writes=[aVb], sembuf=aVb)
                qs, qsb = nxt("q", qb_)
                s.dma("sp", [(qs[:, 0:4, :], qT[0:4, :, lb * 128:(lb + 1) * 128].rearrange("c p n -> p c n"))], writes=[qsb], sembuf=qsb)
                ab = accb[0]
                abl = [accb[0], accb[1]]
                for kb in range(lo, imax + 1):
                    d = imax - kb
                    kl = kb - lo
                    sc, scb = nxt("sc", sc2)
                    for h in range(8):
                        pp = 64 * (h % 2)
                        s.op("pe", lambda e, sc=sc, h=h, pp=pp, kl=kl, qs=qs, kv=kv: e.matmul(
                            sc[:, (h % 2) * 512 + (h // 2) * 128:(h % 2) * 512 + (h // 2) * 128 + 128], kv[pp:pp + 64, h // 2, kl * 128:(kl + 1) * 128],
                            qs[pp:pp + 64, h // 2, :], start=True, stop=True), reads=[aKb, qsb], writes=[scb])
                    mk = cb[:, CB_MB + (par * 24 + d) * 128:CB_MB + (par * 24 + d + 1) * 128]
                    exp_mask_av(sc, scb, 8, 128, lambda hh, d=d, par=par: cf[:, CF_BB + (hh * 2 + par) * 24 + d:CF_BB + (hh * 2 + par) * 24 + d + 1],
                                mk, cbb, lambda j, kl=kl, vvw=vvw: vvw[:, kl, j * 65:(j + 1) * 65],
                                lambda j: accC[:, j * 128:j * 128 + 65], abl, kb == lo, sc_off=lambda h: (h % 2) * 512 + (h // 2) * 128)
                sm_, smb = nxt("sm", sm)
                og, ogb = nxt("stg", stg)
                accv = accC[:, :].rearrange("p (h d) -> p h d", h=8)
                s.op("dve", lambda e, sm_=sm_, accv=accv: e.reciprocal(out=sm_[:, 0:8], in_=accv[:, :, 64]), reads=abl, writes=[smb])
                for h in range(8):
                    s.op("dve", lambda e, sm_=sm_, h=h, og=og: e.tensor_scalar(out=og[:, h * 64:(h + 1) * 64], in0=accC[:, h * 128:h * 128 + 64],
                                                                         scalar1=sm_[:, h:h + 1], scalar2=None, op0=ALU.mult), reads=abl + [smb], writes=[ogb])
                s.dma("sp", [(mixo[lb * 128:(lb + 1) * 128, 0:512], og[:, 0:512])], reads=[ogb], writes=[b_mix], sembuf=ogb)
        if do_c:
            kcd = cx.din("kcT", [128, 1024], BF16)
            vcd = cx.din("vcmp", [128, 1040], BF16)
            kcT = cx.sb([128, 1024], BF16); kcb = Buf("kcT")
            vcmp = cx.sb([128, 8, 2, 65], BF16); vcb = Buf("vcmp")
            s.dma("sp", [(kcT[:], kcd)], writes=[kcb], sembuf=kcb)
            s.dma("sp", [(vcmp[:].rearrange("p a g d -> p (a g d)"), vcd)], writes=[vcb], sembuf=vcb)
            s.dma("sp", [(arenaK[:, :], kT[0])], writes=[aKb], sembuf=aKb)
            avs = arenaV[:, :].rearrange("p (b d) -> p b d", b=128)
            s.dma("sp", [(avs[:, b0:b0 + 8, :], vv[b0 * 128:(b0 + 8) * 128, 0:130].rearrange("(b p) d -> p b d", p=128))
                         for b0 in range(0, 128, 8)], writes=[aVb], sembuf=aVb)
            kwb = cx.sb([128, 12 * 128], BF16); kwbb = Buf("kwb")
            vwb = cx.sb([128, 12, 130], BF16); vwbb = Buf("vwb")
            Ssb = cx.sb([128, 1024], F32); Ssbb = Buf("Ssb")
            Pn = cx.sb([128, 1024], BF16); Pnb = Buf("Pn")
            PnT = cx.sb([128, 1024], BF16); PnTb = Buf("PnT")
            imp = [(cx.sb([128, 1032], F32), Buf("imp")) for _ in range(2)]
            slc = cx.sb([128, 256], F32); slcb = Buf("slc")
            score = cx.sb([128, 256], F32); scoreb = Buf("score")
            score2 = cx.sb([128, 256], F32); score2b = Buf("score2")
            selm = cx.sb([128, 256], BF16); selmb = Buf("selm")
            tmpx = [(cx.sb([128, 128], BF16), Buf("tmpx")) for _ in range(2)]
            gt = cx.sb([128, 48], F32); gtb = Buf("gt")
            gsig = cx.sb([128, 48], F32); gsigb = Buf("gsig")
            ocomb = cx.sb([128, 16, 64], F32); ocb = Buf("ocomb")
            smc = cx.sb([128, 512], F32); smcb = Buf("smc")
            ptp = mb[1][0][:, :].bitcast(BF16)
            for (it, ib) in imp:
                s.op("pool", lambda e, it=it: e.memset(it[:], 0.0), writes=[ib])
            abl = [accb[0], accb[1]]
            accv = accC[:, :].rearrange("p (h d) -> p h d", h=8)
            for lb in slots:
                def _f_lb(lb=lb):
                    par, imin, imax = slot_info(lb)
                    ncol = min(8 * imax + 8, 1023)
                    nj = 2 * imax + 2
                    tdo = CF_TD + par * 1088 + 1016 - 8 * imax
                    zs = 256 - 2 * imax
                    qs, qsb = nxt("q", qb_)
                    s.dma("sp", [(qs[:, :, :], qT[0:8, :, lb * 128:(lb + 1) * 128].rearrange("c p n -> p c n"))], writes=[qsb], sembuf=qsb)
                    s.dma("sp", [(gt[:], gat[lb * 128:(lb + 1) * 128, :])], writes=[gtb], sembuf=gtb)
                    s.op("act", lambda e: e.activation(out=gsig[:], in_=gt[:], func=AF.Sigmoid), reads=[gtb], writes=[gsigb])
                    low = max(0, imin - 4)
                    nkw = imax - low + 1
                    s.dma("sp", [(kwb[:, 0:nkw * 128], kT[1, :, low * 128:(imax + 1) * 128])], writes=[kwbb], sembuf=kwbb)
                    s.dma("sp", [(vwb[:, 0:nkw, :], vv[low * 128:(imax + 1) * 128, 130:260].rearrange("(b p) d -> p b d", p=128))],
                          writes=[vwbb], sembuf=vwbb)
                    for g in range(2):
                        def _f_g(g=g):
                            it, ib = imp[g]
                            for j in range(8):
                                def _f_j(j=j):
                                    h = 8 * g + j
                                    sc, scb = nxt("sc", sc2)
                                    for c0 in range(0, ncol, 512):
                                        c1 = min(ncol, c0 + 512)
                                        s.op("pe", lambda e, sc=sc, c0=c0, c1=c1, g=g, j=j, qs=qs: e.matmul(
                                            sc[:, c0:c1], qs[64 * g:64 * g + 64, j, :], kcT[64 * g:64 * g + 64, c0:c1], start=True, stop=True),
                                            reads=[qsb, kcb], writes=[scb])
                                    s.op("dve", lambda e, sc=sc, h=h: e.scalar_tensor_tensor(out=Ssb[:, 0:ncol], in0=cf[:, tdo:tdo + ncol], scalar=C_SLOPES[h],
                                                                                         in1=sc[:, 0:ncol], op0=ALU.mult, op1=ALU.add),
                                         reads=[scb, cfb], writes=[Ssbb])
                                    s.op("dve", lambda e: e.tensor_reduce(out=smc[:, 0:1], in_=Ssb[:, 0:ncol], axis=AX.X, op=ALU.max, negate=True),
                                         reads=[Ssbb], writes=[smcb])
                                    s.op("dve", lambda e: e.tensor_scalar(out=smc[:, 0:1], in0=smc[:, 0:1], scalar1=1.0e5, scalar2=None, op0=ALU.min),
                                         reads=[smcb], writes=[smcb])
                                    s.op("act", lambda e: e.activation(out=Ssb[:, 0:ncol], in_=Ssb[:, 0:ncol], func=AF.Exp, bias=smc[:, 0:1], accum_out=smc[:, 64:65]),
                                         reads=[Ssbb, smcb], writes=[Ssbb, smcb])
                                    s.op("dve", lambda e: e.tensor_scalar(out=smc[:, 2:3], in0=smc[:, 64:65], scalar1=1.0e-30, scalar2=None, op0=ALU.max),
                                         reads=[smcb], writes=[smcb])
                                    s.op("dve", lambda e: e.reciprocal(out=smc[:, 1:2], in_=smc[:, 2:3]), reads=[smcb], writes=[smcb])
                                    s.op("dve", lambda e: e.tensor_scalar(out=Pn[:, 0:ncol], in0=Ssb[:, 0:ncol], scalar1=smc[:, 1:2], scalar2=None, op0=ALU.mult),
                                         reads=[Ssbb, smcb], writes=[Pnb])
                                    if j == 0:
                                        s.op("dve", lambda e, it=it: e.tensor_scalar(out=it[:, 1:1 + ncol], in0=Ssb[:, 0:ncol], scalar1=smc[:, 1:2], scalar2=None, op0=ALU.mult),
                                             reads=[Ssbb, smcb], writes=[ib])
                                    else:
                                        s.op("dve", lambda e, it=it: e.scalar_tensor_tensor(out=it[:, 1:1 + ncol], in0=Ssb[:, 0:ncol], scalar=smc[:, 1:2], in1=it[:, 1:1 + ncol],
                                                                                         op0=ALU.mult, op1=ALU.add), reads=[Ssbb, smcb, ib], writes=[ib])
                                    nch = (ncol + 127) // 128
                                    for c in range(nch):
                                        w_ = min(128, ncol - c * 128)
                                        s.op("pe", lambda e, c=c, w_=w_: e.transpose(out=ptp[0:w_, c * 128:(c + 1) * 128], in_=Pn[:, c * 128:c * 128 + w_], identity=ident),
                                             reads=[Pnb, cbb], writes=[mb[1][1]])
                                    s.op("act", lambda e, nch=nch: e.activation(out=PnT[:, 0:nch * 128], in_=ptp[:, 0:nch * 128], func=AF.Copy),
                                         reads=[mb[1][1]], writes=[PnTb])
                                    for c in range(nch):
                                        w_ = min(128, ncol - c * 128)
                                        s.op("pe", lambda e, c=c, w_=w_, g=g, nch=nch: e.matmul(mb[0][0][:, 0:64], PnT[0:w_, c * 128:(c + 1) * 128], vcmp[0:w_, c, g, 0:64],
                                                                                        start=(c == 0), stop=(c == nch - 1)), reads=[PnTb, vcb], writes=[mb[0][1]])
                                    s.op("dve", lambda e, h=h: e.tensor_scalar(out=ocomb[:, h, :], in0=mb[0][0][:, 0:64], scalar1=gsig[:, 3 * h:3 * h + 1], scalar2=None, op0=ALU.mult),
                                         reads=[mb[0][1], gsigb], writes=[ocb])
                                _f_j()
                            s.op("dve", lambda e, it=it: e.tensor_reduce(out=slc[:, 0:nj], in_=it[:, 0:4 * nj].rearrange("p (j o) -> p j o", o=4), axis=AX.X, op=ALU.add),
                                 reads=[ib], writes=[slcb])
                            s.op("dve", lambda e, it=it: e.tensor_tensor(out=slc[:, 0:nj], in0=slc[:, 0:nj], in1=it[:, 4:4 * nj + 1:4], op=ALU.add),
                                 reads=[ib, slcb], writes=[slcb])
                            tao = CF_TA + par * 544 + zs
                            tfo = CF_TF + par * 544 + zs
                            s.op("pool", lambda e: e.memset(score[:], -1.0), writes=[scoreb])
                            s.op("dve", lambda e: e.tensor_tensor(out=slc[:, 0:nj], in0=slc[:, 0:nj], in1=cf[:, tao:tao + nj], op=ALU.mult), reads=[slcb, cfb], writes=[slcb])
                            s.op("dve", lambda e: e.scalar_tensor_tensor(out=slc[:, 0:nj], in0=cf[:, tao:tao + nj], scalar=-1.0, in1=slc[:, 0:nj], op0=ALU.add, op1=ALU.add),
                                 reads=[slcb, cfb], writes=[slcb])
                            s.op("dve", lambda e: e.tensor_tensor(out=score[:, 0:nj], in0=slc[:, 0:nj], in1=cf[:, tfo:tfo + nj], op=ALU.max), reads=[slcb, cfb], writes=[scoreb])
                            s.op("dve", lambda e: e.memset(score[:, 0:1], 1.0e6), reads=[], writes=[scoreb])
                            s.op("dve", lambda e: e.max(out=smc[:, 128:136], in_=score[:]), reads=[scoreb], writes=[smcb])
                            s.op("dve", lambda e: e.match_replace(out=score2[:], in_to_replace=smc[:, 128:136], in_values=score[:], imm_value=-2.0),
                                 reads=[scoreb, smcb], writes=[score2b])
                            s.op("dve", lambda e: e.max(out=smc[:, 160:168], in_=score2[:]), reads=[score2b], writes=[smcb])
                            s.op("dve", lambda e: e.tensor_scalar(out=selm[:], in0=score[:], scalar1=smc[:, 167:168], scalar2=None, op0=ALU.is_ge),
                                 reads=[scoreb, smcb], writes=[selmb])
                            for br in (1, 2):
                                def _f_br(br=br):
                                    if br == 1:
                                        kbs = list(range(0, imax + 1))
                                    else:
                                        kbs = list(range(low, imax + 1))
                                    for kb in kbs:
                                        def _f_kb(kb=kb):
                                            d = imax - kb
                                            sc, scb = nxt("sc", sc2)
                                            for a in range(2):
                                                if br == 1:
                                                    lhs = arenaK[64 * g:64 * g + 64, kb * 128:(kb + 1) * 128]
                                                    rds = [aKb, qsb]
                                                else:
                                                    lhs = kwb[64 * g:64 * g + 64, (kb - low) * 128:(kb - low + 1) * 128]
                                                    rds = [kwbb, qsb]
                                                s.op("pe", lambda e, sc=sc, a=a, lhs=lhs, g=g, qs=qs: e.matmul(
                                                    sc[:, a * 512:(a + 1) * 512], lhs, qs[64 * g:64 * g + 64, 4 * a:4 * a + 4, :], start=True, stop=True),
                                                    reads=rds, writes=[scb])
                                            if br == 1:
                                                tx, txb = nxt("md", tmpx)
                                                mt, mtb = nxt("mb", mb)
                                                s.op("pool", lambda e, tx=tx, kb=kb: e.tensor_copy(out=tx[:].rearrange("p (a b) -> p a b", a=2),
                                                                                                 in_=selm[:, 2 * kb:2 * kb + 2].unsqueeze(2).broadcast_to([128, 2, 64])),
                                                     reads=[selmb], writes=[txb])
                                                s.op("pe", lambda e, tx=tx, mt=mt: e.matmul(mt[:, 0:128], tx[:, :], ident, start=True, stop=True),
                                                     reads=[txb, cbb], writes=[mtb])
                                                if d < 8:
                                                    mdt, mdb = nxt("md", md)
                                                    mo = CB_MA + (par * 8 + d) * 128
                                                    s.op("dve", lambda e, mdt=mdt, mt=mt, mo=mo: e.tensor_tensor(out=mdt[:], in0=mt[:, 0:128], in1=cb[:, mo:mo + 128], op=ALU.mult),
                                                         reads=[mtb, cbb], writes=[mdb])
                                                    mask_ap, mask_buf = mdt[:, :], mdb
                                                else:
                                                    mask_ap, mask_buf = mt[:, 0:128], mtb
                                                bb = CF_BC
                                                nd = 136
                                                av_of = (lambda j, kb=kb, g=g: avs[:, kb, g * 65:(g + 1) * 65])
                                                rdv = aVb
                                            else:
                                                mo = CB_MW + (par * 12 + d) * 128
                                                mask_ap, mask_buf = cb[:, mo:mo + 128], cbb
                                                bb = CF_BW
                                                nd = 12
                                                av_of = (lambda j, kb=kb, g=g: vwb[:, kb - low, g * 65:(g + 1) * 65])
                                                rdv = vwbb
                                            exp_mask_av(sc, scb, 8, 128,
                                                        lambda hh, bb=bb, nd=nd, d=d, g=g, par=par: cf[:, bb + ((8 * g + hh) * 2 + par) * nd + d:bb + ((8 * g + hh) * 2 + par) * nd + d + 1],
                                                        mask_ap, mask_buf, av_of, lambda j: accC[:, j * 128:j * 128 + 65], abl, kb == kbs[0], vbuf=rdv)
                                        _f_kb()
                                    s.op("dve", lambda e: e.reciprocal(out=smc[:, 192:200], in_=accv[:, :, 64]), reads=abl, writes=[smcb])
                                    s.op("dve", lambda e, g=g, br=br: e.tensor_tensor(out=smc[:, 192:200], in0=smc[:, 192:200],
                                                                                    in1=gsig[:, 24 * g + br:24 * g + 24:3], op=ALU.mult), reads=[smcb, gsigb], writes=[smcb])
                                    for j in range(8):
                                        def _f_j(j=j):
                                            h = 8 * g + j
                                            s.op("dve", lambda e, j=j, h=h: e.scalar_tensor_tensor(out=ocomb[:, h, :], in0=accC[:, j * 128:j * 128 + 64], scalar=smc[:, 192 + j:193 + j],
                                                                                                 in1=ocomb[:, h, :], op0=ALU.mult, op1=ALU.add),
                                                 reads=abl + [smcb, ocb], writes=[ocb])
                                        _f_j()
                                _f_br()
                        _f_g()
                    og, ogb = nxt("stg", stg)
                    s.op("act", lambda e, og=og: e.activation(out=og[:], in_=ocomb[:].rearrange("p h d -> p (h d)"), func=AF.Copy), reads=[ocb], writes=[ogb])
                    s.dma("sp", [(mixo[lb * 128:(lb + 1) * 128, :], og[:])], reads=[ogb], writes=[b_mix], sembuf=ogb)
                _f_lb()
        s.finish([b_mix])
        s.emit(st)
    return nc


def build_k2p():
    nc = bass.Bass("TRN2", target_bir_lowering=False)
    with ExitStack() as st:
        cx = Ctx(nc, st)
        s = Sched(nc)
        kT = cx.din("kT", [2, 128, S], BF16)
        w1d = [cx.din("w1k", [2048, 128], F32), cx.din("w1v", [2048, 128], F32)]
        w2d = [cx.din("w2k", [128, 64], F32), cx.din("w2v", [128, 64], F32)]
        posd = [cx.din("poskT", [64, 32], F32), cx.din("posvT", [64, 32], F32)]
        kco = cx.dout("kcT", [128, 1024], BF16)
        vco = cx.dout("vcmp", [128, 1040], BF16)
        b_k, b_v = Buf("kco"), Buf("vco")
        cin = [(cx.sb([128, 8208], BF16), Buf("cin")) for _ in range(2)]
        w1 = [(cx.sb([128, 32, 128], BF16), Buf("w1")) for _ in range(2)]
        w2 = [(cx.sb([128, 64], BF16), Buf("w2")) for _ in range(2)]
        pos = [(cx.sb([64, 32], BF16), Buf("pos")) for _ in range(2)]
        hid = [(cx.sb([128, 1024], BF16), Buf("hid")) for _ in range(2)]
        hb = cx.sb([128, 128], F32); hbb = Buf("hb")
        kc = cx.sb([128, 1024], BF16); kcb = Buf("kc")
        vc = cx.sb([128, 8, 2, 65], BF16); vcb = Buf("vc")
        ps = [(cx.ps([128, 512], F32), Buf("ps")) for _ in range(4)]
        pm = [(cx.ps([128, 512], F32), Buf("pm")) for _ in range(2)]
        s.op("pool", lambda e: e.memset(vc[:], 1.0), writes=[vcb])
        s.op("pool", lambda e: e.memset(kc[:], 0.0), writes=[kcb])
        for g in range(2):
            s.op("pool", lambda e, g=g: e.memset(hid[g][0][:], 0.0), writes=[hid[g][1]])
        ev = 0
        for kvi in range(2):
            w1t, w1b = w1[kvi]
            w2t, w2b = w2[kvi]
            pt_, pb_ = pos[kvi]
            w1v_ = w1d[kvi].rearrange("(p d) h -> d p h", d=64)
            s.dma("pool", [(w1t[0:64], w1v_), (w1t[64:128], w1v_)], writes=[w1b], sembuf=w1b)
            s.dma("pool", [(w2t[:], w2d[kvi])], writes=[w2b], sembuf=w2b)
            s.dma("pool", [(pt_[:], posd[kvi])], writes=[pb_], sembuf=pb_)
            p, pb = rr(ev, pm); ev += 1
            for pp in range(32):
                s.op("pe", lambda e, p=p, pp=pp, w1t=w1t, pt_=pt_: e.matmul(p[:, 0:1], w1t[0:64, pp, :], pt_[0:64, pp:pp + 1],
                                                                       start=(pp == 0), stop=(pp == 31)), reads=[w1b, pb_], writes=[pb])
            s.op("dve", lambda e, p=p, kvi=kvi: e.tensor_copy(out=hb[:, 32 * kvi:32 * kvi + 1], in_=p[:, 0:1]), reads=[pb], writes=[hbb])
            for nch in range(2):
                n0, cnt = nch * 512, (512 if nch == 0 else 511)
                ci, cib = cin[nch]
                ntok = 16 * (cnt - 1) + 32
                s.dma("sp", [(ci[:, 0:ntok], kT[kvi, :, 16 * n0:16 * n0 + ntok])], writes=[cib], sembuf=cib)
                for g in range(2):
                    p, pb = rr(ev, ps); ev += 1
                    for pp in range(32):
                        s.op("pe", lambda e, p=p, pp=pp, g=g, ci=ci, cnt=cnt, w1t=w1t: e.matmul(
                            p[:, 0:cnt], w1t[64 * g:64 * g + 64, pp, :], ci[64 * g:64 * g + 64, pp:pp + 16 * (cnt - 1) + 1:16],
                            start=(pp == 0), stop=(pp == 31)), reads=[w1b, cib], writes=[pb])
                    s.op("act", lambda e, p=p, g=g, n0=n0, cnt=cnt, kvi=kvi: e.activation(
                        out=hid[g][0][:, n0:n0 + cnt], in_=p[:, 0:cnt], func=AF.Silu, bias=hb[:, 32 * kvi:32 * kvi + 1]),
                        reads=[pb, hbb], writes=[hid[g][1]])
            for g in range(2):
                ht, htb = hid[g]
                if kvi == 0:
                    for nch in range(2):
                        p, pb = rr(ev, pm); ev += 1
                        s.op("pe", lambda e, p=p, g=g, nch=nch, ht=ht, w2t=w2t: e.matmul(
                            p[64 * g:64 * g + 64, :], w2t[:, :], ht[:, nch * 512:(nch + 1) * 512], start=True, stop=True),
                            reads=[w2b, htb], writes=[pb])
                        s.op("dve", lambda e, p=p, g=g, nch=nch: e.tensor_copy(out=kc[64 * g:64 * g + 64, nch * 512:(nch + 1) * 512],
                                                                            in_=p[64 * g:64 * g + 64, :]), reads=[pb], writes=[kcb])
                else:
                    for c8 in range(8):
                        p, pb = rr(ev, pm); ev += 1
                        s.op("pe", lambda e, p=p, c8=c8, ht=ht, w2t=w2t: e.matmul(
                            p[:, 0:64], ht[:, c8 * 128:(c8 + 1) * 128], w2t[:, :], start=True, stop=True),
                            reads=[w2b, htb], writes=[pb])
                        s.op("dve", lambda e, p=p, c8=c8, g=g: e.tensor_copy(out=vc[:, c8, g, 0:64], in_=p[:, 0:64]),
                             reads=[pb], writes=[vcb])
        s.dma("sp", [(kco, kc[:])], reads=[kcb], writes=[b_k], sembuf=kcb)
        s.dma("sp", [(vco, vc[:].rearrange("p a g d -> p (a g d)"))], reads=[vcb], writes=[b_v], sembuf=vcb)
        s.finish([b_k, b_v])
        s.emit(st)
    return nc


_PROGS = {}
_DEBUG_LAYERS = 0


def _prog(name, fn):
    if name not in _PROGS:
        _PROGS[name] = fn()
    return _PROGS[name]


def _run(nc, in_maps):
    res = run_bass_kernel_spmd(nc, in_maps, core_ids=list(range(NCORES)))
    return res.results


def own_rows(c):
    return np.concatenate([np.arange(gblock(c, lb) * 128, gblock(c, lb) * 128 + 128) for lb in range(16)])


def kernel(x, ln_attn, w_in, w_out, lam_q1, lam_k1, lam_q2, lam_k2, subln,
           cmp_pos_k, cmp_w1_k, cmp_w2_k, cmp_pos_v, cmp_w1_v, cmp_w2_v,
           ln_ffn, ffn_w_gate, ffn_w_up, ffn_w_down,
           router_w, router_b, exp_w_gate, exp_w_up, exp_w_down, ln_final):
    f32 = lambda a: np.ascontiguousarray(np.asarray(a, dtype=np.float32))
    x = f32(x)[0]
    rows = [own_rows(c) for c in range(NCORES)]
    xs = [np.ascontiguousarray(x[r]) for r in rows]
    idn = ident_np()
    consts = [k2_consts(c) for c in range(NCORES)]
    depth = np.asarray(ln_attn).shape[0]
    xf = None
    for l in range(depth):
        r1 = _run(_prog("k1", build_k1), [{"x": xs[c], "g": f32(ln_attn[l:l + 1]), "w": f32(w_in[l]), "ident": idn} for c in range(NCORES)])
        featT = [np.asarray(r["featT"]) for r in r1]
        kfull = np.empty((12, 128, S), dtype=NPBF)
        vfull = np.empty((S, VCOLS), dtype=NPBF)
        kids = [4, 5, 6, 7, 12, 13, 14, 15, 24, 25, 26, 27]
        for c in range(NCORES):
            kfull[:, :, rows[c]] = featT[c][kids]
            vfull[rows[c]] = np.asarray(r1[c]["tokM"])
        imp = {"kT": np.ascontiguousarray(kfull[8:10]), "w1k": f32(cmp_w1_k[l]), "w1v": f32(cmp_w1_v[l]),
               "w2k": f32(cmp_w2_k[l]), "w2v": f32(cmp_w2_v[l]),
               "poskT": np.ascontiguousarray(f32(cmp_pos_k[l]).T), "posvT": np.ascontiguousarray(f32(cmp_pos_v[l]).T)}
        rp = _run(_prog("k2p", build_k2p), [imp] * NCORES)
        kcT, vcmp = np.asarray(rp[0]["kcT"]), np.asarray(rp[0]["vcmp"])
        lam_init = 0.8 - 0.6 * math.exp(-0.3 * l)
        lamv = np.concatenate([f32(lam_q1[l]), f32(lam_k1[l]), f32(lam_q2[l]), f32(lam_k2[l])])[None]
        lcst = np.array([[lam_init, 1.0 - lam_init]], np.float32)
        kA, vA = np.ascontiguousarray(kfull[0:4]), np.ascontiguousarray(vfull[:, 0:516])
        kB, vB = np.ascontiguousarray(kfull[4:8]), np.ascontiguousarray(vfull[:, 516:1036])
        kC, vC = np.ascontiguousarray(kfull[10:12]), np.ascontiguousarray(vfull[:, 1036:1296])
        ra = _run(_prog("k2a", lambda: build_k2(do_a=True, do_b=False, do_c=False)),
                  [{"kT": kA, "vv": vA, "qT": np.ascontiguousarray(featT[c][0:4]), "cb": consts[c][0], "cf": consts[c][1],
                    "lamv": lamv, "lcst": lcst, "subln": f32(subln[l:l + 1])} for c in range(NCORES)])
        rb = _run(_prog("k2b", lambda: build_k2(do_a=False, do_b=True, do_c=False)),
                  [{"kT": kB, "vv": vB, "qT": np.ascontiguousarray(featT[c][8:12]), "cb": consts[c][0], "cf": consts[c][1]}
                   for c in range(NCORES)])
        rc = _run(_prog("k2c", lambda: build_k2(do_a=False, do_b=False, do_c=True)),
                  [{"kT": kC, "vv": vC, "qT": np.ascontiguousarray(featT[c][16:24]), "gat": np.asarray(r1[c]["gates"]),
                    "cb": consts[c][0], "cf": consts[c][1], "kcT": kcT, "vcmp": vcmp} for c in range(NCORES)])
        mix = [np.ascontiguousarray(np.concatenate([np.asarray(ra[c]["mixo"]), np.asarray(rb[c]["mixo"]), np.asarray(rc[c]["mixo"])], axis=1))
               for c in range(NCORES)]
        j = l // 2
        if l % 2 == 0:
            common = {"wo": f32(w_out[l]), "gf": f32(ln_ffn[l:l + 1]), "ident": idn,
                      "wg": f32(ffn_w_gate[j:j + 1]), "wu": f32(ffn_w_up[j:j + 1]), "wd": f32(ffn_w_down[j:j + 1])}
            r3 = _run(_prog("k3d", lambda: build_k3(1)), [dict(common, x=xs[c], mix=mix[c]) for c in range(NCORES)])
        else:
            common = {"wo": f32(w_out[l]), "gf": f32(ln_ffn[l:l + 1]), "ident": idn,
                      "wg": f32(exp_w_gate[j]), "wu": f32(exp_w_up[j]), "wd": f32(exp_w_down[j]),
                      "wr": f32(router_w[j]), "rb": f32(router_b[j:j + 1]), "gfin": f32(ln_final)[None]}
            r3 = _run(_prog("k3m", lambda: build_k3(8)), [dict(common, x=xs[c], mix=mix[c]) for c in range(NCORES)])
            xf = [np.asarray(r["xf"]) for r in r3]
        xs = [np.asarray(r["xo"]) for r in r3]
        if _DEBUG_LAYERS and l + 1 == _DEBUG_LAYERS:
            return xs, rows
    out = np.empty((S, D), np.float32)
    for c in range(NCORES):
        out[rows[c]] = xf[c]
    return out[None]
```

```python
import math
from contextlib import ExitStack
import numpy as np
import ml_dtypes
import concourse.bass as bass
import concourse.mybir as mybir
from concourse.bass_utils import run_bass_kernel_spmd

F32 = mybir.dt.float32
BF16 = mybir.dt.bfloat16
AF = mybir.ActivationFunctionType
ALU = mybir.AluOpType
AX = mybir.AxisListType
NPBF = ml_dtypes.bfloat16

NCORES = 8
D = 2048
S = 16384
NBLK = 128
DFF = 5632
EPS = 1e-6
INCOLS = 4912


def gblock(c, lb):
    return 16 * (lb // 2) + (c if lb % 2 == 0 else 15 - c)


class Buf:
    __slots__ = ("name", "w", "r", "sem", "semval")

    def __init__(self, name=""):
        self.name = name
        self.w = None
        self.r = {}
        self.sem = None
        self.semval = 0


class Sched:
    ENGS = ("pe", "act", "dve", "pool", "sp")

    def __init__(self, nc):
        self.nc = nc
        self.ops = {e: [] for e in self.ENGS}
        self.cnt = {e: 0 for e in self.ENGS}
        self.seen = {e: {} for e in self.ENGS}
        self.nd = 0
        self.outbufs = []

    def _deps(self, eng, reads, writes):
        best = {}
        for b in reads:
            if b.w is not None:
                k, v = b.w
                if best.get(k, 0) < v:
                    best[k] = v
        for b in writes:
            if b.w is not None:
                k, v = b.w
                if best.get(k, 0) < v:
                    best[k] = v
            for k, v in b.r.items():
                if best.get(k, 0) < v:
                    best[k] = v
        seen = self.seen[eng]
        waits = []
        for k, v in best.items():
            if k == eng:
                if eng == "pe" or self.cnt[eng] - v >= 8:
                    continue
            if seen.get(k, 0) >= v:
                continue
            seen[k] = v
            waits.append((k, v))
        return waits

    def _mark(self, tok, reads, writes):
        k, v = tok
        for b in reads:
            if b.r.get(k, 0) < v:
                b.r[k] = v
        for b in writes:
            b.w = tok
            b.r = {}

    def op(self, eng, fn, reads=(), writes=()):
        waits = self._deps(eng, reads, writes)
        self.cnt[eng] += 1
        tok = (eng, self.cnt[eng])
        self.ops[eng].append((waits, fn, tok))
        self._mark(tok, reads, writes)
        return tok

    def dma(self, queue, pairs, reads=(), writes=(), sembuf=None):
        waits = self._deps(queue, reads, writes)
        if sembuf.sem is None:
            sembuf.sem = self.nd
            self.nd += 1
        key = ("d", sembuf.sem)
        for i, (o, a) in enumerate(pairs):
            sembuf.semval += 16
            self.ops[queue].append((waits if i == 0 else [],
                                    (lambda e, o=o, a=a: e.dma_start(out=o, in_=a)),
                                    (key, sembuf.semval)))
        tok = (key, sembuf.semval)
        self._mark(tok, reads, writes)
        return tok

    def realias(self, new_bufs, old_bufs):
        acc = {}
        for b in old_bufs:
            if b.w is not None:
                k, v = b.w
                acc[k] = max(acc.get(k, 0), v)
            for k, v in b.r.items():
                acc[k] = max(acc.get(k, 0), v)
        for b in new_bufs:
            b.w = None
            b.r = dict(acc)

    def finish(self, outbufs):
        self.op("sp", lambda e: "nop", reads=outbufs)

    def emit(self, st):
        nc = self.nc
        esem = {e: st.enter_context(nc.semaphore("s_" + e)) for e in self.ENGS}
        dsem = [st.enter_context(nc.semaphore("d%d" % i)) for i in range(self.nd)]
        block = st.enter_context(nc.Block())

        def mk(name):
            def body(e):
                for waits, fn, tok in self.ops[name]:
                    for k, v in waits:
                        e.wait_ge(esem[k] if isinstance(k, str) else dsem[k[1]], v)
                    ins = fn(e)
                    if isinstance(ins, str):
                        continue
                    assert ins is not None, ("builder returned None", name, tok)
                    if isinstance(tok[0], str):
                        ins.then_inc(esem[tok[0]], 1)
                    else:
                        ins.then_inc(dsem[tok[0][1]], 16)
            return body

        block.tensor(mk("pe"))
        block.scalar(mk("act"))
        block.vector(mk("dve"))
        block.gpsimd(mk("pool"))
        block.sync(mk("sp"))


class Ctx:
    def __init__(self, nc, st):
        self.nc = nc
        self.st = st
        self.n = 0

    def sb(self, shape, dt, name=None):
        self.n += 1
        return self.st.enter_context(self.nc.sbuf_tensor(name or ("t%d" % self.n), list(shape), dt))

    def ps(self, shape, dt, name=None):
        self.n += 1
        return self.st.enter_context(self.nc.psum_tensor(name or ("p%d" % self.n), list(shape), dt))

    def din(self, name, shape, dt):
        return self.nc.dram_tensor(name, list(shape), dt, kind="ExternalInput").ap()

    def dout(self, name, shape, dt):
        return self.nc.dram_tensor(name, list(shape), dt, kind="ExternalOutput").ap()


def rr(i, lst):
    return lst[i % len(lst)]


def pipelined(items, front, back):
    pend = front(items[0]) if items else None
    for i, it in enumerate(items):
        cur = pend
        pend = front(items[i + 1]) if i + 1 < len(items) else None
        back(it, cur)


def emit_rmsnorm_T(s, cx, src_ap, src_buf, gbc, gbuf, hT, hTbuf, tcol, ident, identb, wk, scale_dim=D):
    i = wk["i"]
    wk["i"] += 1
    junk, junkb = rr(i, wk["junk"])
    ss, ssb = rr(i, wk["ss"])
    hb, hbb = rr(i, wk["hb"])
    s.op("act", lambda e: e.activation(out=junk[:], in_=src_ap, func=AF.Square, accum_out=ss[:, 0:1]),
         reads=[src_buf], writes=[junkb, ssb])
    s.op("dve", lambda e: e.tensor_scalar(out=ss[:, 32:33], in0=ss[:, 0:1], scalar1=1.0 / scale_dim, scalar2=EPS,
                                          op0=ALU.mult, op1=ALU.add), reads=[ssb], writes=[ssb])
    s.op("act", lambda e: e.activation(out=ss[:, 64:65], in_=ss[:, 32:33], func=AF.Sqrt), reads=[ssb], writes=[ssb])
    s.op("dve", lambda e: e.reciprocal(out=ss[:, 96:97], in_=ss[:, 64:65]), reads=[ssb], writes=[ssb])
    s.op("dve", lambda e: e.scalar_tensor_tensor(out=hb[:], in0=src_ap, scalar=ss[:, 96:97], in1=gbc[:],
                                                 op0=ALU.mult, op1=ALU.mult),
         reads=[src_buf, ssb, gbuf], writes=[hbb])
    for half in range(2):
        ptr, ptrb = rr(2 * i + half, wk["ptr"])
        for k in range(8):
            dc = half * 8 + k
            s.op("pe", lambda e, dc=dc, k=k, ptr=ptr: e.transpose(out=ptr[:, k * 128:(k + 1) * 128],
                                                                 in_=hb[:, dc * 128:(dc + 1) * 128], identity=ident[:]),
                 reads=[hbb, identb], writes=[ptrb])
        dst = hT[:, half * 8:half * 8 + 8, tcol:tcol + 128]
        srcp = ptr[:].rearrange("p (a b) -> p a b", a=8)
        if (i + half) % 2 == 0:
            s.op("act", lambda e, dst=dst, srcp=srcp: e.activation(out=dst, in_=srcp, func=AF.Copy),
                 reads=[ptrb], writes=[hTbuf])
        else:
            s.op("dve", lambda e, dst=dst, srcp=srcp: e.tensor_copy(out=dst, in_=srcp),
                 reads=[ptrb], writes=[hTbuf])


def mk_norm_work(cx):
    wk = {"i": 0}
    wk["junk"] = [(cx.sb([128, D], BF16), Buf("junk"))]
    wk["ss"] = [(cx.sb([128, 128], F32), Buf("ss")) for _ in range(2)]
    wk["hb"] = [(cx.sb([128, D], BF16), Buf("hb")) for _ in range(2)]
    wk["ptr"] = [(cx.ps([128, 1024], BF16), Buf("ptr")) for _ in range(2)]
    return wk


def k1_chunks():
    ch = []
    for h in range(4):
        ch.append(([(0 + 64 * h, 64), (256 + 64 * h, 64)], 0.125))
    for h in range(4):
        ch.append(([(512 + 64 * h, 64), (768 + 64 * h, 64)], 1.0))
    for c in range(4):
        ch.append(([(1536 + 128 * c, 128)], 0.125))
    for c in range(4):
        ch.append(([(2048 + 128 * c, 128)], 1.0))
    for j in range(8):
        ch.append(([(3072 + 64 * j, 64), (3072 + 64 * (8 + j), 64)], 0.125))
    for c0 in (4096, 4224, 4352, 4608):
        ch.append(([(c0, 128)], 1.0))
    return ch


NFCH = 28
VCOLS = 1296
TOKCOLS = [(1024, 512), (2560, 512), (4480, 128), (4736, 128), (4864, 48)]


def build_k1(ntiles=16):
    nc = bass.Bass("TRN2", target_bir_lowering=False)
    NT = ntiles * 128
    with ExitStack() as st:
        cx = Ctx(nc, st)
        s = Sched(nc)
        x = cx.din("x", [NT, D], F32)
        g = cx.din("g", [1, D], F32)
        w = cx.din("w", [D, INCOLS], F32)
        idn = cx.din("ident", [128, 128], BF16)
        featT = cx.dout("featT", [NFCH, 128, NT], BF16)
        tokM = cx.dout("tokM", [NT, VCOLS], BF16)
        gates = cx.dout("gates", [NT, 48], F32)
        b_featT, b_tokM, b_gates = Buf("featT"), Buf("tokM"), Buf("gates")

        ident = cx.sb([128, 128], BF16); identb = Buf("ident")
        gbc = cx.sb([128, D], F32); gbuf = Buf("gbc")
        xt = [(cx.sb([128, D], F32), Buf("xt")) for _ in range(2)]
        hT = cx.sb([128, 16, NT], BF16); hTb = Buf("hT")
        wtok = cx.sb([128, 16, 1328], BF16); wtokb = Buf("wtok")
        wst = [(cx.sb([128, 16, 128], BF16), Buf("wst")) for _ in range(2)]
        fst = [(cx.sb([128, NT], BF16), Buf("fst")) for _ in range(2)]
        tst = [(cx.sb([128, VCOLS], BF16), Buf("tst")) for _ in range(2)]
        gst = [(cx.sb([128, 48], F32), Buf("gst")) for _ in range(2)]
        wk = mk_norm_work(cx)
        pf = [(cx.ps([128, 512], F32), Buf("pf")) for _ in range(3)]
        pt = [(cx.ps([128, 512], F32), Buf("pt")) for _ in range(3)]

        s.dma("sp", [(ident[:], idn)], writes=[identb], sembuf=identb)
        s.dma("sp", [(gbc[:], g.partition_broadcast(128))], writes=[gbuf], sembuf=gbuf)
        for (tt, tb_) in tst:
            s.op("pool", lambda e, tt=tt: e.memset(tt[:], 1.0), writes=[tb_])
        wv = w.rearrange("(dc p) c -> p dc c", p=128)
        off = 0
        pairs = []
        for c0, ln in TOKCOLS:
            pairs.append((wtok[:, :, off:off + ln], wv[:, :, c0:c0 + ln]))
            off += ln
        s.dma("pool", pairs, writes=[wtokb], sembuf=wtokb)
        for t in range(ntiles):
            xtt, xtb = rr(t, xt)
            s.dma("sp", [(xtt[:], x[t * 128:(t + 1) * 128, :])], writes=[xtb], sembuf=xtb)
            emit_rmsnorm_T(s, cx, xtt[:], xtb, gbc, gbuf, hT, hTb, t * 128, ident, identb, wk)
        ntg = (NT + 511) // 512
        ev = 0
        for c, (segs, scale) in enumerate(k1_chunks()):
            wt, wtb = rr(c, wst)
            pairs = []
            off = 0
            for c0, ln in segs:
                pairs.append((wt[:, :, off:off + ln], wv[:, :, c0:c0 + ln]))
                off += ln
            s.dma("pool", pairs, writes=[wtb], sembuf=wtb)
            fs, fsb = rr(c, fst)
            for tg in range(ntg):
                n = min(512, NT - tg * 512)
                p, pb = rr(ev, pf)
                for dc in range(16):
                    s.op("pe", lambda e, p=p, wt=wt, dc=dc, tg=tg, n=n: e.matmul(
                        p[:, 0:n], wt[:, dc, :], hT[:, dc, tg * 512:tg * 512 + n], start=(dc == 0), stop=(dc == 15)),
                        reads=[wtb, hTb], writes=[pb])
                if ev % 2 == 0:
                    s.op("act", lambda e, p=p, fs=fs, tg=tg, n=n, scale=scale: e.activation(
                        out=fs[:, tg * 512:tg * 512 + n], in_=p[:, 0:n], func=AF.Copy, scale=scale),
                        reads=[pb], writes=[fsb])
                else:
                    s.op("dve", lambda e, p=p, fs=fs, tg=tg, n=n, scale=scale: e.tensor_scalar(
                        out=fs[:, tg * 512:tg * 512 + n], in0=p[:, 0:n], scalar1=scale, scalar2=None, op0=ALU.mult),
                        reads=[pb], writes=[fsb])
                ev += 1
            s.dma("sp", [(featT[c], fs[:])], reads=[fsb], writes=[b_featT], sembuf=fsb)
        for t in range(ntiles):
            ts_, tsb = rr(t, tst)
            gs, gsb = rr(t, gst)
            for gi, (c0, n) in enumerate([(0, 512), (512, 512), (1024, 304)]):
                p, pb = rr(ev, pt)
                for dc in range(16):
                    s.op("pe", lambda e, p=p, dc=dc, t=t, c0=c0, n=n: e.matmul(
                        p[:, 0:n], hT[:, dc, t * 128:(t + 1) * 128], wtok[:, dc, c0:c0 + n],
                        start=(dc == 0), stop=(dc == 15)), reads=[wtokb, hTb], writes=[pb])
                if gi == 0:
                    mv = [(ts_[:, 0:516].rearrange("p (h d) -> p h d", h=4)[:, :, 0:128], p[:, 0:512].rearrange("p (h d) -> p h d", h=4))]
                elif gi == 1:
                    mv = [(ts_[:, 516:1036].rearrange("p (h d) -> p h d", h=8)[:, :, 0:64], p[:, 0:512].rearrange("p (h d) -> p h d", h=8))]
                else:
                    mv = [(ts_[:, 1036:1166].rearrange("p (h d) -> p h d", h=2)[:, :, 0:64], p[:, 0:128].rearrange("p (h d) -> p h d", h=2)),
                          (ts_[:, 1166:1296].rearrange("p (h d) -> p h d", h=2)[:, :, 0:64], p[:, 128:256].rearrange("p (h d) -> p h d", h=2)),
                          (gs[:], p[:, 256:304])]
                for (o_, i_) in mv:
                    wb_ = gsb if o_ is mv[-1][0] and gi == 2 else tsb
                    if t % 2 == 0:
                        s.op("act", lambda e, o_=o_, i_=i_: e.activation(out=o_, in_=i_, func=AF.Copy), reads=[pb], writes=[wb_])
                    else:
                        s.op("dve", lambda e, o_=o_, i_=i_: e.tensor_copy(out=o_, in_=i_), reads=[pb], writes=[wb_])
                ev += 1
            s.dma("sp", [(tokM[t * 128:(t + 1) * 128, :], ts_[:])], reads=[tsb], writes=[b_tokM], sembuf=tsb)
            s.dma("sp", [(gates[t * 128:(t + 1) * 128, :], gs[:])], reads=[gsb], writes=[b_gates], sembuf=gsb)
        s.finish([b_featT, b_tokM, b_gates])
        s.emit(st)
    return nc


def ident_np():
    return np.eye(128, dtype=np.float32).astype(NPBF)


def build_k3(nexp, nhalf=2, nslot=DFF // 256):
    nc = bass.Bass("TRN2", target_bir_lowering=False)
    moe = nexp > 1
    NT = nhalf * 1024
    with ExitStack() as st:
        cx = Ctx(nc, st)
        s = Sched(nc)
        x = cx.din("x", [NT, D], F32)
        mix = cx.din("mix", [NT, D], BF16)
        wo = cx.din("wo", [D, D], F32)
        gf = cx.din("gf", [1, D], F32)
        idn = cx.din("ident", [128, 128], BF16)
        wg = cx.din("wg", [nexp, D, DFF], F32)
        wu = cx.din("wu", [nexp, D, DFF], F32)
        wd = cx.din("wd", [nexp, DFF, D], F32)
        xo = cx.dout("xo", [NT, D], F32)
        b_xo = Buf("xo")
        outs = [b_xo]
        if moe:
            wr = cx.din("wr", [D, 8], F32)
            rb = cx.din("rb", [1, 8], F32)
            gfin = cx.din("gfin", [1, D], F32)
            xf = cx.dout("xf", [NT, D], F32)
            b_xf = Buf("xf")
            outs.append(b_xf)

        ident = cx.sb([128, 128], BF16); identb = Buf("ident")
        gbc = cx.sb([128, D], F32); gbuf = Buf("gbc")
        yacc = cx.sb([128, 8, D], F32); yb = [Buf("y%d" % i) for i in range(8)]
        hT = cx.sb([128, 16, 1024], BF16); hTb = Buf("hT")
        wk = {"i": 0}
        wk["junk"] = [(cx.sb([128, D], BF16), Buf("junk"))]
        wk["ss"] = [(cx.sb([128, 128], F32), Buf("ss")) for _ in range(2)]
        wk["hb"] = [(cx.sb([128, D], BF16), Buf("hb"))]
        wk["ptr"] = [(cx.ps([128, 1024], BF16), Buf("ptr")) for _ in range(2)]
        ring = cx.sb([128, 2, 12288], BF16)
        slot_b = [Buf("slot0"), Buf("slot1")]
        f1_b = [Buf("mixt0"), Buf("mixt1"), Buf("xpc0"), Buf("xpc1"), Buf("wos0"), Buf("wos1")]

        def slot_views(k):
            base = ring[:, k, :]
            wgv = base[:, 0:4096].rearrange("p (a b) -> p a b", a=16)
            wuv = base[:, 4096:8192].rearrange("p (a b) -> p a b", a=16)
            wdv = base[:, 8192:12288].rearrange("p (a b) -> p a b", a=2)
            return wgv, wuv, wdv
        mixt = [ring[:, 0, 0:2048], ring[:, 0, 2048:4096]]
        xpc = [ring[:, 0, 4096:5120].bitcast(F32), ring[:, 0, 5120:6144].bitcast(F32)]
        wos = [ring[:, 0, 6144:12288], ring[:, 1, 0:6144]]
        OCG = 384
        ocgs = [(c0, min(OCG, D - c0)) for c0 in range(0, D, OCG)]
        actT = [(cx.sb([128, 1024], BF16), Buf("actT")) for _ in range(2)]
        sg = [(cx.sb([128, 1024], BF16), Buf("sg")) for _ in range(2)]
        pG = [(cx.ps([128, 1024], F32), Buf("pG"))]
        pU = [(cx.ps([128, 1024], F32), Buf("pU"))]
        pD = [(cx.ps([128, 512], F32), Buf("pD")) for _ in range(2)]
        allps = [pG[0], pU[0]] + pD
        if moe:
            wrb = cx.sb([128, 16, 8], BF16); wrbb = Buf("wrb")
            rbbc = cx.sb([128, 8], F32); rbb = Buf("rbbc")
            call = cx.sb([128, 8, 8], F32); cb = [Buf("c%d" % i) for i in range(8)]
            rt = [(cx.sb([128, 192], F32), Buf("rt")) for _ in range(2)]
            s.dma("pool", [(wrb[:], wr.rearrange("(dc p) c -> p dc c", p=128))], writes=[wrbb], sembuf=wrbb)
            s.dma("sp", [(rbbc[:], rb.partition_broadcast(128))], writes=[rbb], sembuf=rbb)
        s.dma("sp", [(ident[:], idn)], writes=[identb], sembuf=identb)
        wov = wo.rearrange("(dc p) c -> p dc c", p=128)
        ev = 0
        for hf in range(nhalf):
            r0 = hf * 1024
            s.dma("sp", [(gbc[:], gf.partition_broadcast(128))], writes=[gbuf], sembuf=gbuf)
            s.realias(f1_b, slot_b)
            for t in range(8):
                mt, mtb = mixt[t % 2], f1_b[t % 2]
                s.dma("sp", [(mt, mix[r0 + t * 128:r0 + (t + 1) * 128, :])], writes=[mtb], sembuf=mtb)
                for half in range(2):
                    ptr, ptrb = rr(2 * t + half, wk["ptr"])
                    for k in range(8):
                        dc = half * 8 + k
                        s.op("pe", lambda e, dc=dc, k=k, ptr=ptr, mt=mt: e.transpose(
                            out=ptr[:, k * 128:(k + 1) * 128], in_=mt[:, dc * 128:(dc + 1) * 128], identity=ident[:]),
                            reads=[mtb, identb], writes=[ptrb])
                    dst = hT[:, half * 8:half * 8 + 8, t * 128:(t + 1) * 128]
                    srcp = ptr[:].rearrange("p (a b) -> p a b", a=8)
                    if half == 0:
                        s.op("act", lambda e, dst=dst, srcp=srcp: e.activation(out=dst, in_=srcp, func=AF.Copy),
                             reads=[ptrb], writes=[hTb])
                    else:
                        s.op("dve", lambda e, dst=dst, srcp=srcp: e.tensor_copy(out=dst, in_=srcp),
                             reads=[ptrb], writes=[hTb])
            for ci, (c0, cw) in enumerate(ocgs):
                wv_, wb_ = wos[ci % 2], f1_b[4 + ci % 2]
                wv3 = wv_.rearrange("p (a b) -> p a b", a=16)
                s.dma("pool", [(wv3[:, :, 0:cw], wov[:, :, c0:c0 + cw])], writes=[wb_], sembuf=wb_)
                for t in range(8):
                    xp, xpb = xpc[ev % 2], f1_b[2 + ev % 2]
                    s.dma("sp", [(xp[:, 0:cw], x[r0 + t * 128:r0 + (t + 1) * 128, c0:c0 + cw])], writes=[xpb], sembuf=xpb)
                    p, pb = rr(ev, allps)
                    for dc in range(16):
                        s.op("pe", lambda e, p=p, dc=dc, t=t, wv3=wv3, cw=cw: e.matmul(
                            p[:, 0:cw], hT[:, dc, t * 128:(t + 1) * 128], wv3[:, dc, 0:cw],
                            start=(dc == 0), stop=(dc == 15)), reads=[hTb, wb_], writes=[pb])
                    s.op("dve", lambda e, p=p, t=t, c0=c0, cw=cw, xp=xp: e.tensor_tensor(
                        out=yacc[:, t, c0:c0 + cw], in0=p[:, 0:cw], in1=xp[:, 0:cw], op=ALU.add),
                        reads=[pb, xpb], writes=[yb[t]])
                    ev += 1
            for t in range(8):
                emit_rmsnorm_T(s, cx, yacc[:, t, :], yb[t], gbc, gbuf, hT, hTb, t * 128, ident, identb, wk)
            if moe:
                for t in range(8):
                    p, pb = rr(t, pD)
                    r_, rb_ = rr(t, rt)
                    for dc in range(16):
                        s.op("pe", lambda e, p=p, dc=dc, t=t: e.matmul(
                            p[:, 0:8], hT[:, dc, t * 128:(t + 1) * 128], wrb[:, dc, :],
                            start=(dc == 0), stop=(dc == 15)), reads=[hTb, wrbb], writes=[pb])
                    s.op("dve", lambda e, p=p, r_=r_: e.tensor_tensor(out=r_[:, 0:8], in0=p[:, 0:8], in1=rbbc[:], op=ALU.add),
                         reads=[pb, rbb], writes=[rb_])
                    s.op("dve", lambda e, r_=r_: e.max(out=r_[:, 8:16], in_=r_[:, 0:8]), reads=[rb_], writes=[rb_])
                    s.op("dve", lambda e, r_=r_: e.tensor_tensor(out=r_[:, 16:17], in0=r_[:, 9:10], in1=r_[:, 8:9], op=ALU.subtract),
                         reads=[rb_], writes=[rb_])
                    s.op("act", lambda e, r_=r_: e.activation(out=r_[:, 128:129], in_=r_[:, 16:17], func=AF.Exp),
                         reads=[rb_], writes=[rb_])
                    s.op("dve", lambda e, r_=r_: e.tensor_scalar(out=r_[:, 18:19], in0=r_[:, 128:129], scalar1=1.0, scalar2=None, op0=ALU.add),
                         reads=[rb_], writes=[rb_])
                    s.op("dve", lambda e, r_=r_: e.reciprocal(out=r_[:, 19:20], in_=r_[:, 18:19]), reads=[rb_], writes=[rb_])
                    s.op("dve", lambda e, r_=r_: e.tensor_tensor(out=r_[:, 20:21], in0=r_[:, 128:129], in1=r_[:, 19:20], op=ALU.mult),
                         reads=[rb_], writes=[rb_])
                    s.op("dve", lambda e, r_=r_: e.tensor_scalar(out=r_[:, 24:32], in0=r_[:, 0:8], scalar1=r_[:, 8:9], scalar2=r_[:, 19:20],
                                                                op0=ALU.is_equal, op1=ALU.mult), reads=[rb_], writes=[rb_])
                    s.op("dve", lambda e, r_=r_: e.tensor_scalar(out=r_[:, 32:40], in0=r_[:, 0:8], scalar1=r_[:, 9:10], scalar2=r_[:, 20:21],
                                                                op0=ALU.is_equal, op1=ALU.mult), reads=[rb_], writes=[rb_])
                    s.op("dve", lambda e, r_=r_, t=t: e.tensor_tensor(out=call[:, t, :], in0=r_[:, 24:32], in1=r_[:, 32:40], op=ALU.add),
                         reads=[rb_], writes=[cb[t]])
            s.realias(slot_b, f1_b)
            G, Gb = pG[0]
            U, Ub = pU[0]
            pending = None
            units = [(t, dg) for t in range(8) for dg in range(4)]

            def down(pend, lo, hi):
                nonlocal ev
                (wdv, slb, acts, e_) = pend
                for (t, dg) in units[lo:hi]:
                    p, pb = rr(ev, pD)
                    for a in range(2):
                        at, atb = acts[a]
                        s.op("pe", lambda e, p=p, at=at, wdv=wdv, a=a, t=t, dg=dg: e.matmul(
                            p[:, :], at[:, t * 128:(t + 1) * 128], wdv[:, a, dg * 512:(dg + 1) * 512],
                            start=(a == 0), stop=(a == 1)), reads=[atb, slb], writes=[pb])
                    if moe:
                        s.op("dve", lambda e, p=p, t=t, dg=dg, e_=e_: e.scalar_tensor_tensor(
                            out=yacc[:, t, dg * 512:(dg + 1) * 512], in0=p[:, :], scalar=call[:, t, e_:e_ + 1],
                            in1=yacc[:, t, dg * 512:(dg + 1) * 512], op0=ALU.mult, op1=ALU.add),
                            reads=[pb, cb[t], yb[t]], writes=[yb[t]])
                    else:
                        s.op("dve", lambda e, p=p, t=t, dg=dg: e.tensor_tensor(
                            out=yacc[:, t, dg * 512:(dg + 1) * 512], in0=p[:, :],
                            in1=yacc[:, t, dg * 512:(dg + 1) * 512], op=ALU.add),
                            reads=[pb, yb[t]], writes=[yb[t]])
                    ev += 1

            k = 0
            for e_ in range(nexp):
                wgv_d = wg[e_].rearrange("(dc p) f -> p dc f", p=128)
                wuv_d = wu[e_].rearrange("(dc p) f -> p dc f", p=128)
                for sl in range(nslot):
                    f0 = sl * 256
                    wgv, wuv, wdv = slot_views(k % 2)
                    slb = slot_b[k % 2]
                    s.dma("pool", [(wgv, wgv_d[:, :, f0:f0 + 256]), (wuv, wuv_d[:, :, f0:f0 + 256]),
                                   (wdv, wd[e_, f0:f0 + 256, :].rearrange("(a p) d -> p a d", p=128))],
                          writes=[slb], sembuf=slb)
                    for a in range(2):
                        for (P_, Pb_, wv_) in ((G, Gb, wgv), (U, Ub, wuv)):
                            for tg in range(2):
                                for dc in range(16):
                                    s.op("pe", lambda e, P_=P_, wv_=wv_, dc=dc, tg=tg, a=a: e.matmul(
                                        P_[:, tg * 512:(tg + 1) * 512], wv_[:, dc, a * 128:(a + 1) * 128],
                                        hT[:, dc, tg * 512:(tg + 1) * 512], start=(dc == 0), stop=(dc == 15)),
                                        reads=[slb, hTb], writes=[Pb_])
                        sg_, sgb = sg[a]
                        at, atb = actT[a]
                        if pending is not None and a == 0:
                            down(pending, 0, 32)
                            pending = None
                        s.op("act", lambda e, sg_=sg_: e.activation(out=sg_[:], in_=G[:, :], func=AF.Silu),
                             reads=[Gb], writes=[sgb])
                        s.op("dve", lambda e, sg_=sg_, at=at: e.tensor_tensor(out=at[:], in0=sg_[:], in1=U[:, :], op=ALU.mult),
                             reads=[sgb, Ub], writes=[atb])
                    pending = (wdv, slb, [actT[0], actT[1]], e_)
                    k += 1
            down(pending, 0, 32)
            pending = None
            if moe:
                s.dma("sp", [(gbc[:], gfin.partition_broadcast(128))], writes=[gbuf], sembuf=gbuf)
            for t in range(8):
                s.dma("sp", [(xo[r0 + t * 128:r0 + (t + 1) * 128, :], yacc[:, t, :])], reads=[yb[t]], writes=[b_xo], sembuf=yb[t])
                if moe:
                    ss, ssb = rr(t, wk["ss"])
                    junk, junkb = wk["junk"][0]
                    s.op("act", lambda e, t=t, ss=ss, junk=junk: e.activation(out=junk[:], in_=yacc[:, t, :], func=AF.Square, accum_out=ss[:, 0:1]),
                         reads=[yb[t]], writes=[junkb, ssb])
                    s.op("dve", lambda e, ss=ss: e.tensor_scalar(out=ss[:, 32:33], in0=ss[:, 0:1], scalar1=1.0 / D, scalar2=EPS,
                                                                 op0=ALU.mult, op1=ALU.add), reads=[ssb], writes=[ssb])
                    s.op("act", lambda e, ss=ss: e.activation(out=ss[:, 64:65], in_=ss[:, 32:33], func=AF.Sqrt), reads=[ssb], writes=[ssb])
                    s.op("dve", lambda e, ss=ss: e.reciprocal(out=ss[:, 96:97], in_=ss[:, 64:65]), reads=[ssb], writes=[ssb])
                    s.op("dve", lambda e, t=t, ss=ss: e.scalar_tensor_tensor(out=yacc[:, t, :], in0=yacc[:, t, :], scalar=ss[:, 96:97], in1=gbc[:],
                                                                           op0=ALU.mult, op1=ALU.mult),
                         reads=[ssb, gbuf, yb[t]], writes=[yb[t]])
                    s.dma("sp", [(xf[r0 + t * 128:r0 + (t + 1) * 128, :], yacc[:, t, :])], reads=[yb[t]], writes=[b_xf], sembuf=yb[t])
        s.finish(outs)
        s.emit(st)
    return nc


A_SLOPES = [2.0 ** (-8.0 * (i + 1) / 4) for i in range(4)]
B_SLOPES = [2.0 ** (-8.0 * (i + 1) / 8) for i in range(8)]
C_SLOPES = [2.0 ** (-8.0 * (i + 1) / 16) for i in range(16)]
NEGB = -30000.0


def dskip(slope):
    return int(math.ceil(8 + 150.0 / (128.0 * slope))) + 1
CB_ID, CB_CAUS, CB_MA, CB_MW, CB_MB, CB_N = 0, 128, 256, 256 + 2048, 256 + 2048 + 3072, 256 + 2048 + 3072 + 6144
CF_TD, CF_TA, CF_TF = 0, 2176, 2176 + 1088
CF_BC = CF_TF + 1088
CF_BW = CF_BC + 16 * 2 * 136
CF_BA = CF_BW + 16 * 2 * 12
CF_BB = CF_BA + 4 * 2 * 136
CF_N = CF_BB + 8 * 2 * 24


def slot_info(lb):
    par, m = lb % 2, lb // 2
    imin = 16 * m + (0 if par == 0 else 8)
    return par, imin, imin + 7


def k2_consts(c):
    k = np.arange(128)[:, None]
    q = np.arange(128)[None, :]
    caus = (k <= q).astype(np.float32)
    anti = (k > q).astype(np.float32)
    cb = np.zeros((128, CB_N), np.float32)
    cb[:, CB_ID:CB_ID + 128] = np.eye(128)
    cb[:, CB_CAUS:CB_CAUS + 128] = caus
    cf = np.zeros((128, CF_N), np.float32)
    qi = np.arange(128)[:, None]
    for par in range(2):
        e = (7 - c) if par == 0 else c
        for d in range(8):
            dl = d - e
            mk = np.zeros((128, 128)) if dl < 0 else (caus if dl == 0 else np.ones((128, 128)))
            cb[:, CB_MA + (par * 8 + d) * 128:CB_MA + (par * 8 + d + 1) * 128] = mk
        for d in range(12):
            dl = d - e
            mk = caus if dl == 0 else (np.ones((128, 128)) if 1 <= dl <= 3 else (anti if dl == 4 else np.zeros((128, 128))))
            cb[:, CB_MW + (par * 12 + d) * 128:CB_MW + (par * 12 + d + 1) * 128] = mk
        for d in range(24):
            dl = d - e
            if 0 <= dl <= 16:
                dist = 128 * dl + q - k
                ok = (dist >= 0)
                mk = (ok & (dist <= 128)).astype(np.float32) + (ok & (dist % 4 == 0) & (dist <= 512)) + (ok & (dist % 16 == 0) & (dist <= 2048))
            else:
                mk = np.zeros((128, 128))
            cb[:, CB_MB + (par * 24 + d) * 128:CB_MB + (par * 24 + d + 1) * 128] = mk
        z = np.arange(1088)[None, :]
        mm = z - 1016 + 8 * e
        dd = qi - 16 * mm - 31
        cf[:, CF_TD + par * 1088:CF_TD + (par + 1) * 1088] = np.where(dd >= 0, -dd, -1e9)
        z = np.arange(544)[None, :]
        y = z - 256 + 2 * e
        allow = (y <= 0) | ((y == 1) & (qi >= 64))
        forced = (y == 0) | ((y == -1) & (qi < 64)) | ((y == 1) & (qi >= 64))
        cf[:, CF_TA + par * 544:CF_TA + (par + 1) * 544] = allow
        cf[:, CF_TF + par * 544:CF_TF + (par + 1) * 544] = np.where(forced, 1e6, -1.0)
        kk = np.arange(128)[:, None]
        for (base, slopes, nd, lo, hi) in ((CF_BA, A_SLOPES, 136, 0, 127), (CF_BC, C_SLOPES, 136, 0, 127),
                                           (CF_BW, C_SLOPES, 12, 0, 4), (CF_BB, B_SLOPES, 24, 0, 16)):
            for h, sl in enumerate(slopes):
                d = np.arange(nd)[None, :]
                dl = d - e
                val = sl * (kk - 64 - 128 * dl)
                val = np.where((dl >= lo) & (dl <= hi), val, NEGB)
                o = base + (h * 2 + par) * nd
                cf[:, o:o + nd] = val
    return cb.astype(NPBF), cf.astype(np.float32)


def build_k2(slots=tuple(range(16)), do_a=True, do_b=True, do_c=True):
    nc = bass.Bass("TRN2", target_bir_lowering=False)
    NS = 16
    NQ = NS * 128
    with ExitStack() as st:
        cx = Ctx(nc, st)
        s = Sched(nc)
        nkc, nvc, nqc = (4, 516, 4) if do_a else ((4, 520, 4) if do_b else (2, 260, 8))
        kT = cx.din("kT", [nkc, 128, S], BF16)
        vv = cx.din("vv", [S, nvc], BF16)
        qT = cx.din("qT", [nqc, 128, NQ], BF16)
        if do_c:
            gat = cx.din("gat", [NQ, 48], F32)
        cbd = cx.din("cb", [128, CB_N], BF16)
        cfd = cx.din("cf", [128, CF_N], F32)
        if do_a:
            lamv = cx.din("lamv", [1, 256], F32)
            lcst = cx.din("lcst", [1, 2], F32)
            subln = cx.din("subln", [1, 128], F32)
        ocols = 512 if (do_a or do_b) else 1024
        mixo = cx.dout("mixo", [NQ, ocols], BF16)
        b_mix = Buf("mixo")

        cbn = CB_MW if do_a else (CB_N if do_b else CB_MB)
        cf0, cf1 = (CF_BA, CF_BB) if do_a else ((CF_BB, CF_N) if do_b else (0, CF_BA))
        cb = cx.sb([128, cbn], BF16); cbb = Buf("cb")
        cf_t = cx.sb([128, cf1 - cf0], F32); cfb = Buf("cf")
        s.dma("sp", [(cb[:], cbd[:, 0:cbn])], writes=[cbb], sembuf=cbb)
        s.dma("sp", [(cf_t[:], cfd[:, cf0:cf1])], writes=[cfb], sembuf=cfb)

        class _CF:
            def __getitem__(self, idx):
                p_, c_ = idx
                return cf_t[p_, slice(c_.start - cf0, c_.stop - cf0)]
        cf = _CF()
        ident = cb[:, CB_ID:CB_ID + 128]
        arenaK = cx.sb([128, S], BF16); aKb = Buf("arenaK")
        arenaV = cx.sb([128, 128 * 130], BF16); aVb = Buf("arenaV")
        sc2 = [(cx.ps([128, 1024], F32), Buf("sc")) for _ in range(2)]
        accC = cx.ps([128, 1024], F32)
        accb = [Buf("acc0"), Buf("acc1")]
        mb = [(cx.ps([128, 512], F32), Buf("mb")) for _ in range(2)]
        PT = [(cx.sb([128, 8, 128], BF16), Buf("PT")) for _ in range(3)]
        md = [(cx.sb([128, 128], BF16), Buf("md")) for _ in range(2)]
        qb_ = [(cx.sb([128, 8, 128], BF16), Buf("q")) for _ in range(2)]
        stg = [(cx.sb([128, 1024], BF16), Buf("stg")) for _ in range(2)]
        sm = [(cx.sb([128, 512], F32), Buf("sm")) for _ in range(2)]
        nev = {"sc": 0, "pt": 0, "md": 0, "md2": 0, "tx": 0, "q": 0, "stg": 0, "sm": 0, "mb": 0}

        def nxt(key, lst):
            i = nev[key]
            nev[key] += 1
            return lst[i % len(lst)]

        def exp_mask_av(sc, scb, nh, ncols, bias_of, mask_ap, mask_buf, av_of, acc_of, accbuf, started, per_bank=4, vbuf=None, sc_off=lambda h: h * 128, h0=0):
            pt, ptb = nxt("pt", PT)
            for h in range(h0, nh):
                s.op("act", lambda e, h=h, pt=pt, sc=sc: e.activation(
                    out=pt[:, h, 0:ncols] if ncols == 128 else pt[:, 2 * h:2 * h + 2, :],
                    in_=sc[:, sc_off(h):sc_off(h) + 128] if ncols == 128 else sc[:, :].rearrange("p (a b) -> p a b", a=2)[:, :, 0:128],
                    func=AF.Exp, bias=bias_of(h)), reads=[scb, cfb], writes=[ptb])
            nt = nh * (ncols // 128)
            j0_ = h0 * (ncols // 128)
            if mask_ap is not None:
                s.op("dve", lambda e, pt=pt: e.tensor_tensor(
                    out=pt[:, j0_:nt, :], in0=pt[:, j0_:nt, :], in1=mask_ap.unsqueeze(1).broadcast_to([128, nt - j0_, 128]), op=ALU.mult),
                    reads=[mask_buf, ptb], writes=[ptb])
            for j in range(j0_, nt):
                st_ = (j // per_bank) not in started
                started.add(j // per_bank)
                s.op("pe", lambda e, j=j, pt=pt, st_=st_: e.matmul(acc_of(j), pt[:, j, :], av_of(j), start=st_, stop=True,
                                                         skip_group_check=True), reads=[ptb, vbuf or aVb], writes=accbuf)

        if do_a:
            lam_t = cx.sb([128, 512], F32); lamb = Buf("lam")
            gsub = cx.sb([128, 128], F32); gsb_ = Buf("gsub")
            s.dma("sp", [(lam_t[:, 0:256], lamv.partition_broadcast(128)), (lam_t[:, 256:258], lcst.partition_broadcast(128))],
                  writes=[lamb], sembuf=lamb)
            s.dma("sp", [(gsub[:], subln.partition_broadcast(128))], writes=[gsb_], sembuf=gsb_)
            s.op("dve", lambda e: e.tensor_tensor(out=lam_t[:, 0:64], in0=lam_t[:, 0:64], in1=lam_t[:, 64:128], op=ALU.mult), reads=[lamb], writes=[lamb])
            s.op("dve", lambda e: e.tensor_tensor(out=lam_t[:, 128:192], in0=lam_t[:, 128:192], in1=lam_t[:, 192:256], op=ALU.mult), reads=[lamb], writes=[lamb])
            s.op("dve", lambda e: e.tensor_reduce(out=lam_t[:, 288:289], in_=lam_t[:, 0:64], axis=AX.X, op=ALU.add), reads=[lamb], writes=[lamb])
            s.op("dve", lambda e: e.tensor_reduce(out=lam_t[:, 289:290], in_=lam_t[:, 128:192], axis=AX.X, op=ALU.add), reads=[lamb], writes=[lamb])
            s.op("act", lambda e: e.activation(out=lam_t[:, 320:322], in_=lam_t[:, 288:290], func=AF.Exp), reads=[lamb], writes=[lamb])
            s.op("dve", lambda e: e.tensor_tensor(out=lam_t[:, 352:353], in0=lam_t[:, 320:321], in1=lam_t[:, 321:322], op=ALU.subtract), reads=[lamb], writes=[lamb])
            s.op("dve", lambda e: e.tensor_tensor(out=lam_t[:, 353:354], in0=lam_t[:, 352:353], in1=lam_t[:, 256:257], op=ALU.add), reads=[lamb], writes=[lamb])
            s.op("dve", lambda e: e.tensor_scalar(out=lam_t[:, 354:355], in0=lam_t[:, 353:354], scalar1=-1.0, scalar2=None, op0=ALU.mult), reads=[lamb], writes=[lamb])
            s.op("dve", lambda e: e.tensor_scalar(out=gsub[:], in0=gsub[:], scalar1=lam_t[:, 257:258], scalar2=None, op0=ALU.mult), reads=[lamb, gsb_], writes=[gsb_])
            NLAM = 354
            for h in range(4):
                s.dma("sp", [(arenaK[:, :], kT[h])], writes=[aKb], sembuf=aKb)
                avv = arenaV[:, 0:128 * 129].rearrange("p (b d) -> p b d", b=128)
                s.dma("sp", [(avv[:, b0:b0 + 8, :], vv[b0 * 128:(b0 + 8) * 128, h * 129:(h + 1) * 129].rearrange("(b p) d -> p b d", p=128))
                             for b0 in range(0, 128, 8)], writes=[aVb], sembuf=aVb)
                for lb in slots:
                    par, imin, imax = slot_info(lb)
                    qs, qsb = nxt("q", qb_)
                    s.dma("sp", [(qs[:, 0, :], qT[h, :, lb * 128:(lb + 1) * 128])], writes=[qsb], sembuf=qsb)
                    ab = accb[lb % 2]
                    acc = accC[:, (lb % 2) * 512:(lb % 2) * 512 + 512]
                    stA = set()

                    def frontA(kb, qs=qs, qsb=qsb):
                        sc, scb = nxt("sc", sc2)
                        for m_ in range(2):
                            s.op("pe", lambda e, sc=sc, m_=m_, kb=kb, qs=qs: e.matmul(
                                sc[:, m_ * 512:m_ * 512 + 128], arenaK[64 * m_:64 * m_ + 64, kb * 128:(kb + 1) * 128],
                                qs[64 * m_:64 * m_ + 64, 0, :], start=True, stop=True), reads=[aKb, qsb], writes=[scb])
                        return sc, scb

                    def backA(kb, fr, h=h, par=par, imax=imax, avv=avv, acc=acc, ab=ab, stA=stA):
                        sc, scb = fr
                        d = imax - kb
                        bo = CF_BA + (h * 2 + par) * 136 + d
                        mk = cb[:, CB_MA + (par * 8 + d) * 128:CB_MA + (par * 8 + d + 1) * 128] if d < 8 else None
                        exp_mask_av(sc, scb, 1, 256, lambda hh, bo=bo: cf[:, bo:bo + 1], mk, cbb,
                                    lambda j, kb=kb, avv=avv: avv[:, kb, :], lambda j, acc=acc: acc[:, j * 256:j * 256 + 129], [ab], stA)
                    pipelined(list(range(max(0, imax + 1 - dskip(A_SLOPES[h])), imax + 1)), frontA, backA)
                    sm_, smb = nxt("sm", sm)
                    og, ogb = nxt("stg", stg)
                    s.op("dve", lambda e, sm_=sm_, acc=acc: e.reciprocal(out=sm_[:, 0:1], in_=acc[:, 128:129]), reads=[ab], writes=[smb])
                    s.op("dve", lambda e, sm_=sm_, acc=acc: e.reciprocal(out=sm_[:, 1:2], in_=acc[:, 256 + 128:256 + 129]), reads=[ab], writes=[smb])
                    s.op("dve", lambda e, sm_=sm_: e.tensor_tensor(out=sm_[:, 2:3], in0=sm_[:, 1:2], in1=lam_t[:, NLAM:NLAM + 1], op=ALU.mult), reads=[lamb, smb], writes=[smb])
                    s.op("dve", lambda e, sm_=sm_, acc=acc: e.tensor_scalar(out=sm_[:, 128:256], in0=acc[:, 0:128], scalar1=sm_[:, 0:1], scalar2=None, op0=ALU.mult), reads=[ab, smb], writes=[smb])
                    s.op("dve", lambda e, sm_=sm_, acc=acc: e.scalar_tensor_tensor(out=sm_[:, 128:256], in0=acc[:, 256:384], scalar=sm_[:, 2:3], in1=sm_[:, 128:256], op0=ALU.mult, op1=ALU.add), reads=[ab, smb], writes=[smb])
                    s.op("dve", lambda e, sm_=sm_: e.tensor_tensor(out=sm_[:, 256:384], in0=sm_[:, 128:256], in1=sm_[:, 128:256], op=ALU.mult), reads=[smb], writes=[smb])
                    s.op("dve", lambda e, sm_=sm_: e.tensor_reduce(out=sm_[:, 16:17], in_=sm_[:, 256:384], axis=AX.X, op=ALU.add), reads=[smb], writes=[smb])
                    s.op("dve", lambda e, sm_=sm_: e.tensor_scalar(out=sm_[:, 17:18], in0=sm_[:, 16:17], scalar1=1.0 / 128, scalar2=EPS, op0=ALU.mult, op1=ALU.add), reads=[smb], writes=[smb])
                    s.op("act", lambda e, sm_=sm_: e.activation(out=sm_[:, 64:65], in_=sm_[:, 17:18], func=AF.Sqrt), reads=[smb], writes=[smb])
                    s.op("dve", lambda e, sm_=sm_: e.reciprocal(out=sm_[:, 18:19], in_=sm_[:, 64:65]), reads=[smb], writes=[smb])
                    s.op("dve", lambda e, sm_=sm_, og=og: e.scalar_tensor_tensor(out=og[:, 0:128], in0=sm_[:, 128:256], scalar=sm_[:, 18:19], in1=gsub[:], op0=ALU.mult, op1=ALU.mult), reads=[smb, gsb_], writes=[ogb])
                    s.dma("sp", [(mixo[lb * 128:(lb + 1) * 128, h * 128:(h + 1) * 128], og[:, 0:128])], reads=[ogb], writes=[b_mix], sembuf=ogb)

        if do_b:
            for lb in slots:
                par, imin, imax = slot_info(lb)
                lo = max(0, imin - 16)
                nkb = imax - lo + 1
                kv = arenaK[:, 0:4 * nkb * 128].rearrange("p (c n) -> p c n", c=4)
                vvw = arenaV[:, 0:nkb * 520].rearrange("p (b d) -> p b d", b=nkb)
                s.dma("sp", [(kv[:, cch, :], kT[cch, :, lo * 128:(imax + 1) * 128]) for cch in range(4)], writes=[aKb], sembuf=aKb)
                s.dma("sp", [(vvw, vv[lo * 128:(imax + 1) * 128, 0:520].rearrange("(b p) d -> p b d", p=128))], writes=[aVb], sembuf=aVb)
                qs, qsb = nxt("q", qb_)
                s.dma("sp", [(qs[:, 0:4, :], qT[0:4, :, lb * 128:(lb + 1) * 128].rearrange("c p n -> p c n"))], writes=[qsb], sembuf=qsb)
                ab = accb[0]
                abl = [accb[0], accb[1]]
                stB = set()

                def frontB(kb, qs=qs, qsb=qsb, kv=kv, lo=lo):
                    kl = kb - lo
                    sc, scb = nxt("sc", sc2)
                    for h in range(8):
                        pp = 64 * (h % 2)
                        s.op("pe", lambda e, sc=sc, h=h, pp=pp, kl=kl, qs=qs, kv=kv: e.matmul(
                            sc[:, (h % 2) * 512 + (h // 2) * 128:(h % 2) * 512 + (h // 2) * 128 + 128], kv[pp:pp + 64, h // 2, kl * 128:(kl + 1) * 128],
                            qs[pp:pp + 64, h // 2, :], start=True, stop=True), reads=[aKb, qsb], writes=[scb])
                    return sc, scb

                def backB(kb, fr, par=par, imax=imax, lo=lo, vvw=vvw, abl=abl, stB=stB):
                    sc, scb = fr
                    d = imax - kb
                    kl = kb - lo
                    mk = cb[:, CB_MB + (par * 24 + d) * 128:CB_MB + (par * 24 + d + 1) * 128]
                    exp_mask_av(sc, scb, 8, 128, lambda hh, d=d, par=par: cf[:, CF_BB + (hh * 2 + par) * 24 + d:CF_BB + (hh * 2 + par) * 24 + d + 1],
                                mk, cbb, lambda j, kl=kl, vvw=vvw: vvw[:, kl, j * 65:(j + 1) * 65],
                                lambda j: accC[:, j * 128:j * 128 + 65], abl, stB, sc_off=lambda h: (h % 2) * 512 + (h // 2) * 128)
                pipelined(list(range(lo, imax + 1)), frontB, backB)
                sm_, smb = nxt("sm", sm)
                og, ogb = nxt("stg", stg)
                accv = accC[:, :].rearrange("p (h d) -> p h d", h=8)
                s.op("dve", lambda e, sm_=sm_, accv=accv: e.reciprocal(out=sm_[:, 0:8], in_=accv[:, :, 64]), reads=abl, writes=[smb])
                for h in range(8):
                    s.op("dve", lambda e, sm_=sm_, h=h, og=og: e.tensor_scalar(out=og[:, h * 64:(h + 1) * 64], in0=accC[:, h * 128:h * 128 + 64],
                                                                         scalar1=sm_[:, h:h + 1], scalar2=None, op0=ALU.mult), reads=abl + [smb], writes=[ogb])
                s.dma("sp", [(mixo[lb * 128:(lb + 1) * 128, 0:512], og[:, 0:512])], reads=[ogb], writes=[b_mix], sembuf=ogb)
        if do_c:
            kcd = cx.din("kcT", [128, 1024], BF16)
            vcd = cx.din("vcmp", [128, 1040], BF16)
            kcT = cx.sb([128, 1024], BF16); kcb = Buf("kcT")
            vcmp = cx.sb([128, 8, 2, 65], BF16); vcb = Buf("vcmp")
            s.dma("sp", [(kcT[:], kcd)], writes=[kcb], sembuf=kcb)
            s.dma("sp", [(vcmp[:].rearrange("p a g d -> p (a g d)"), vcd)], writes=[vcb], sembuf=vcb)
            s.dma("sp", [(arenaK[:, :], kT[0])], writes=[aKb], sembuf=aKb)
            avs = arenaV[:, :].rearrange("p (b d) -> p b d", b=128)
            s.dma("sp", [(avs[:, b0:b0 + 8, :], vv[b0 * 128:(b0 + 8) * 128, 0:130].rearrange("(b p) d -> p b d", p=128))
                         for b0 in range(0, 128, 8)], writes=[aVb], sembuf=aVb)
            kwb = cx.sb([128, 12 * 128], BF16); kwbb = Buf("kwb")
            vwb = cx.sb([128, 12, 130], BF16); vwbb = Buf("vwb")
            Ssb = cx.sb([128, 1024], F32); Ssbb = Buf("Ssb")
            Pn = cx.sb([128, 1024], BF16); Pnb = Buf("Pn")
            PnT = cx.sb([128, 1024], BF16); PnTb = Buf("PnT")
            imp = [(cx.sb([128, 1032], F32), Buf("imp")) for _ in range(2)]
            slc = cx.sb([128, 256], F32); slcb = Buf("slc")
            score = cx.sb([128, 256], F32); scoreb = Buf("score")
            score2 = cx.sb([128, 256], F32); score2b = Buf("score2")
            selm = cx.sb([128, 256], BF16); selmb = Buf("selm")
            tmpx = [(cx.sb([128, 128], BF16), Buf("tmpx")) for _ in range(2)]
            gt = cx.sb([128, 48], F32); gtb = Buf("gt")
            gsig = cx.sb([128, 48], F32); gsigb = Buf("gsig")
            ocomb = cx.sb([128, 16, 64], F32); ocb = Buf("ocomb")
            smc = cx.sb([128, 512], F32); smcb = Buf("smc")
            ptp = mb[1][0][:, :].bitcast(BF16)
            for (it, ib) in imp:
                s.op("pool", lambda e, it=it: e.memset(it[:], 0.0), writes=[ib])
            abl = [accb[0], accb[1]]
            accv = accC[:, :].rearrange("p (h d) -> p h d", h=8)
            for lb in slots:
                def _f_lb(lb=lb):
                    par, imin, imax = slot_info(lb)
                    ncol = min(8 * imax + 8, 1023)
                    nj = 2 * imax + 2
                    tdo = CF_TD + par * 1088 + 1016 - 8 * imax
                    zs = 256 - 2 * imax
                    qs, qsb = nxt("q", qb_)
                    s.dma("sp", [(qs[:, :, :], qT[0:8, :, lb * 128:(lb + 1) * 128].rearrange("c p n -> p c n"))], writes=[qsb], sembuf=qsb)
                    s.dma("sp", [(gt[:], gat[lb * 128:(lb + 1) * 128, :])], writes=[gtb], sembuf=gtb)
                    s.op("act", lambda e: e.activation(out=gsig[:], in_=gt[:], func=AF.Sigmoid), reads=[gtb], writes=[gsigb])
                    low = max(0, imin - 4)
                    nkw = imax - low + 1
                    s.dma("sp", [(kwb[:, 0:nkw * 128], kT[1, :, low * 128:(imax + 1) * 128])], writes=[kwbb], sembuf=kwbb)
                    s.dma("sp", [(vwb[:, 0:nkw, :], vv[low * 128:(imax + 1) * 128, 130:260].rearrange("(b p) d -> p b d", p=128))],
                          writes=[vwbb], sembuf=vwbb)
                    for g in range(2):
                        def _f_g(g=g):
                            it, ib = imp[g]
                            for j in range(8):
                                def _f_j(j=j):
                                    h = 8 * g + j
                                    sc, scb = nxt("sc", sc2)
                                    for c0 in range(0, ncol, 512):
                                        c1 = min(ncol, c0 + 512)
                                        s.op("pe", lambda e, sc=sc, c0=c0, c1=c1, g=g, j=j, qs=qs: e.matmul(
                                            sc[:, c0:c1], qs[64 * g:64 * g + 64, j, :], kcT[64 * g:64 * g + 64, c0:c1], start=True, stop=True),
                                            reads=[qsb, kcb], writes=[scb])
                                    s.op("dve", lambda e, sc=sc, h=h: e.scalar_tensor_tensor(out=Ssb[:, 0:ncol], in0=cf[:, tdo:tdo + ncol], scalar=C_SLOPES[h],
                                                                                         in1=sc[:, 0:ncol], op0=ALU.mult, op1=ALU.add),
                                         reads=[scb, cfb], writes=[Ssbb])
                                    s.op("dve", lambda e: e.tensor_reduce(out=smc[:, 0:1], in_=Ssb[:, 0:ncol], axis=AX.X, op=ALU.max, negate=True),
                                         reads=[Ssbb], writes=[smcb])
                                    s.op("dve", lambda e: e.tensor_scalar(out=smc[:, 0:1], in0=smc[:, 0:1], scalar1=1.0e5, scalar2=None, op0=ALU.min),
                                         reads=[smcb], writes=[smcb])
                                    s.op("act", lambda e: e.activation(out=Ssb[:, 0:ncol], in_=Ssb[:, 0:ncol], func=AF.Exp, bias=smc[:, 0:1], accum_out=smc[:, 64:65]),
                                         reads=[Ssbb, smcb], writes=[Ssbb, smcb])
                                    s.op("dve", lambda e: e.tensor_scalar(out=smc[:, 2:3], in0=smc[:, 64:65], scalar1=1.0e-30, scalar2=None, op0=ALU.max),
                                         reads=[smcb], writes=[smcb])
                                    s.op("dve", lambda e: e.reciprocal(out=smc[:, 1:2], in_=smc[:, 2:3]), reads=[smcb], writes=[smcb])
                                    s.op("dve", lambda e: e.tensor_scalar(out=Pn[:, 0:ncol], in0=Ssb[:, 0:ncol], scalar1=smc[:, 1:2], scalar2=None, op0=ALU.mult),
                                         reads=[Ssbb, smcb], writes=[Pnb])
                                    if j == 0:
                                        s.op("dve", lambda e, it=it: e.tensor_scalar(out=it[:, 1:1 + ncol], in0=Ssb[:, 0:ncol], scalar1=smc[:, 1:2], scalar2=None, op0=ALU.mult),
                                             reads=[Ssbb, smcb], writes=[ib])
                                    else:
                                        s.op("dve", lambda e, it=it: e.scalar_tensor_tensor(out=it[:, 1:1 + ncol], in0=Ssb[:, 0:ncol], scalar=smc[:, 1:2], in1=it[:, 1:1 + ncol],
                                                                                         op0=ALU.mult, op1=ALU.add), reads=[Ssbb, smcb, ib], writes=[ib])
                                    nch = (ncol + 127) // 128
                                    for c in range(nch):
                                        w_ = min(128, ncol - c * 128)
                                        s.op("pe", lambda e, c=c, w_=w_: e.transpose(out=ptp[0:w_, c * 128:(c + 1) * 128], in_=Pn[:, c * 128:c * 128 + w_], identity=ident),
                                             reads=[Pnb, cbb], writes=[mb[1][1]])
                                    s.op("act", lambda e, nch=nch: e.activation(out=PnT[:, 0:nch * 128], in_=ptp[:, 0:nch * 128], func=AF.Copy),
                                         reads=[mb[1][1]], writes=[PnTb])
                                    for c in range(nch):
                                        w_ = min(128, ncol - c * 128)
                                        s.op("pe", lambda e, c=c, w_=w_, g=g, nch=nch: e.matmul(mb[0][0][:, 0:64], PnT[0:w_, c * 128:(c + 1) * 128], vcmp[0:w_, c, g, 0:64],
                                                                                        start=(c == 0), stop=(c == nch - 1)), reads=[PnTb, vcb], writes=[mb[0][1]])
                                    s.op("dve", lambda e, h=h: e.tensor_scalar(out=ocomb[:, h, :], in0=mb[0][0][:, 0:64], scalar1=gsig[:, 3 * h:3 * h + 1], scalar2=None, op0=ALU.mult),
                                         reads=[mb[0][1], gsigb], writes=[ocb])
                                _f_j()
                            s.op("dve", lambda e, it=it: e.tensor_reduce(out=slc[:, 0:nj], in_=it[:, 0:4 * nj].rearrange("p (j o) -> p j o", o=4), axis=AX.X, op=ALU.add),
                                 reads=[ib], writes=[slcb])
                            s.op("dve", lambda e, it=it: e.tensor_tensor(out=slc[:, 0:nj], in0=slc[:, 0:nj], in1=it[:, 4:4 * nj + 1:4], op=ALU.add),
                                 reads=[ib, slcb], writes=[slcb])
                            tao = CF_TA + par * 544 + zs
                            tfo = CF_TF + par * 544 + zs
                            s.op("pool", lambda e: e.memset(score[:], -1.0), writes=[scoreb])
                            s.op("dve", lambda e: e.tensor_tensor(out=slc[:, 0:nj], in0=slc[:, 0:nj], in1=cf[:, tao:tao + nj], op=ALU.mult), reads=[slcb, cfb], writes=[slcb])
                            s.op("dve", lambda e: e.scalar_tensor_tensor(out=slc[:, 0:nj], in0=cf[:, tao:tao + nj], scalar=-1.0, in1=slc[:, 0:nj], op0=ALU.add, op1=ALU.add),
                                 reads=[slcb, cfb], writes=[slcb])
                            s.op("dve", lambda e: e.tensor_tensor(out=score[:, 0:nj], in0=slc[:, 0:nj], in1=cf[:, tfo:tfo + nj], op=ALU.max), reads=[slcb, cfb], writes=[scoreb])
                            s.op("dve", lambda e: e.memset(score[:, 0:1], 1.0e6), reads=[], writes=[scoreb])
                            s.op("dve", lambda e: e.max(out=smc[:, 128:136], in_=score[:]), reads=[scoreb], writes=[smcb])
                            s.op("dve", lambda e: e.match_replace(out=score2[:], in_to_replace=smc[:, 128:136], in_values=score[:], imm_value=-2.0),
                                 reads=[scoreb, smcb], writes=[score2b])
                            s.op("dve", lambda e: e.max(out=smc[:, 160:168], in_=score2[:]), reads=[score2b], writes=[smcb])
                            s.op("dve", lambda e: e.tensor_scalar(out=selm[:], in0=score[:], scalar1=smc[:, 167:168], scalar2=None, op0=ALU.is_ge),
                                 reads=[scoreb, smcb], writes=[selmb])
                            for br in (1, 2):
                                def _f_br(br=br):
                                    stC = set()

                                    def h0_of(d_):
                                        return sum(1 for j_ in range(8) if d_ >= dskip(C_SLOPES[8 * g + j_])) if br == 1 else 0
                                    if br == 1:
                                        kbs = [kb_ for kb_ in range(0, imax + 1) if h0_of(imax - kb_) < 8]
                                    else:
                                        kbs = list(range(low, imax + 1))
                                    if True:
                                        def _frontC(kb):
                                            d = imax - kb
                                            h0 = h0_of(d)
                                            sc, scb = nxt("sc", sc2)
                                            for a in range(2):
                                                if h0 >= 4 * (a + 1):
                                                    continue
                                                if br == 1:
                                                    lhs = arenaK[64 * g:64 * g + 64, kb * 128:(kb + 1) * 128]
                                                    rds = [aKb, qsb]
                                                else:
                                                    lhs = kwb[64 * g:64 * g + 64, (kb - low) * 128:(kb - low + 1) * 128]
                                                    rds = [kwbb, qsb]
                                                s.op("pe", lambda e, sc=sc, a=a, lhs=lhs, g=g, qs=qs: e.matmul(
                                                    sc[:, a * 512:(a + 1) * 512], lhs, qs[64 * g:64 * g + 64, 4 * a:4 * a + 4, :], start=True, stop=True),
                                                    reads=rds, writes=[scb])
                                            if br == 1:
                                                tx, txb = nxt("tx", tmpx)
                                                mt, mtb = nxt("mb", mb)
                                                s.op("pool", lambda e, tx=tx, kb=kb: e.tensor_copy(out=tx[:].rearrange("p (a b) -> p a b", a=2),
                                                                                                 in_=selm[:, 2 * kb:2 * kb + 2].unsqueeze(2).broadcast_to([128, 2, 64])),
                                                     reads=[selmb], writes=[txb])
                                                s.op("pe", lambda e, tx=tx, mt=mt: e.matmul(mt[:, 0:128], tx[:, :], ident, start=True, stop=True),
                                                     reads=[txb, cbb], writes=[mtb])
                                                if d < 8:
                                                    mdt, mdb = nxt("md2", md)
                                                    mo = CB_MA + (par * 8 + d) * 128
                                                    s.op("dve", lambda e, mdt=mdt, mt=mt, mo=mo: e.tensor_tensor(out=mdt[:], in0=mt[:, 0:128], in1=cb[:, mo:mo + 128], op=ALU.mult),
                                                         reads=[mtb, cbb], writes=[mdb])
                                                    mask_ap, mask_buf = mdt[:, :], mdb
                                                else:
                                                    mask_ap, mask_buf = mt[:, 0:128], mtb
                                                bb = CF_BC
                                                nd = 136
                                                av_of = (lambda j, kb=kb, g=g: avs[:, kb, g * 65:(g + 1) * 65])
                                                rdv = aVb
                                            else:
                                                mo = CB_MW + (par * 12 + d) * 128
                                                mask_ap, mask_buf = cb[:, mo:mo + 128], cbb
                                                bb = CF_BW
                                                nd = 12
                                                av_of = (lambda j, kb=kb, g=g: vwb[:, kb - low, g * 65:(g + 1) * 65])
                                                rdv = vwbb
                                            return (sc, scb, h0, d, bb, nd, mask_ap, mask_buf, av_of, rdv)

                                        def _backC(kb, fr):
                                            (sc, scb, h0, d, bb, nd, mask_ap, mask_buf, av_of, rdv) = fr
                                            exp_mask_av(sc, scb, 8, 128,
                                                        lambda hh, bb=bb, nd=nd, d=d, g=g, par=par: cf[:, bb + ((8 * g + hh) * 2 + par) * nd + d:bb + ((8 * g + hh) * 2 + par) * nd + d + 1],
                                                        mask_ap, mask_buf, av_of, lambda j: accC[:, j * 128:j * 128 + 65], abl, stC, vbuf=rdv, h0=h0)
                                        pipelined(kbs, _frontC, _backC)
                                    s.op("dve", lambda e: e.reciprocal(out=smc[:, 192:200], in_=accv[:, :, 64]), reads=abl, writes=[smcb])
                                    s.op("dve", lambda e, g=g, br=br: e.tensor_tensor(out=smc[:, 192:200], in0=smc[:, 192:200],
                                                                                    in1=gsig[:, 24 * g + br:24 * g + 24:3], op=ALU.mult), reads=[smcb, gsigb], writes=[smcb])
                                    for j in range(8):
                                        def _f_j(j=j):
                                            h = 8 * g + j
                                            s.op("dve", lambda e, j=j, h=h: e.scalar_tensor_tensor(out=ocomb[:, h, :], in0=accC[:, j * 128:j * 128 + 64], scalar=smc[:, 192 + j:193 + j],
                                                                                                 in1=ocomb[:, h, :], op0=ALU.mult, op1=ALU.add),
                                                 reads=abl + [smcb, ocb], writes=[ocb])
                                        _f_j()
                                _f_br()
                        _f_g()
                    og, ogb = nxt("stg", stg)
                    s.op("act", lambda e, og=og: e.activation(out=og[:], in_=ocomb[:].rearrange("p h d -> p (h d)"), func=AF.Copy), reads=[ocb], writes=[ogb])
                    s.dma("sp", [(mixo[lb * 128:(lb + 1) * 128, :], og[:])], reads=[ogb], writes=[b_mix], sembuf=ogb)
                _f_lb()
        s.finish([b_mix])
        s.emit(st)
    return nc


def build_k2p():
    nc = bass.Bass("TRN2", target_bir_lowering=False)
    with ExitStack() as st:
        cx = Ctx(nc, st)
        s = Sched(nc)
        kT = cx.din("kT", [2, 128, S], BF16)
        w1d = [cx.din("w1k", [2048, 128], F32), cx.din("w1v", [2048, 128], F32)]
        w2d = [cx.din("w2k", [128, 64], F32), cx.din("w2v", [128, 64], F32)]
        posd = [cx.din("poskT", [64, 32], F32), cx.din("posvT", [64, 32], F32)]
        kco = cx.dout("kcT", [128, 1024], BF16)
        vco = cx.dout("vcmp", [128, 1040], BF16)
        b_k, b_v = Buf("kco"), Buf("vco")
        cin = [(cx.sb([128, 8208], BF16), Buf("cin")) for _ in range(2)]
        w1 = [(cx.sb([128, 32, 128], BF16), Buf("w1")) for _ in range(2)]
        w2 = [(cx.sb([128, 64], BF16), Buf("w2")) for _ in range(2)]
        pos = [(cx.sb([64, 32], BF16), Buf("pos")) for _ in range(2)]
        hid = [(cx.sb([128, 1024], BF16), Buf("hid")) for _ in range(2)]
        hb = cx.sb([128, 128], F32); hbb = Buf("hb")
        kc = cx.sb([128, 1024], BF16); kcb = Buf("kc")
        vc = cx.sb([128, 8, 2, 65], BF16); vcb = Buf("vc")
        ps = [(cx.ps([128, 512], F32), Buf("ps")) for _ in range(4)]
        pm = [(cx.ps([128, 512], F32), Buf("pm")) for _ in range(2)]
        s.op("pool", lambda e: e.memset(vc[:], 1.0), writes=[vcb])
        s.op("pool", lambda e: e.memset(kc[:], 0.0), writes=[kcb])
        for g in range(2):
            s.op("pool", lambda e, g=g: e.memset(hid[g][0][:], 0.0), writes=[hid[g][1]])
        ev = 0
        for kvi in range(2):
            w1t, w1b = w1[kvi]
            w2t, w2b = w2[kvi]
            pt_, pb_ = pos[kvi]
            w1v_ = w1d[kvi].rearrange("(p d) h -> d p h", d=64)
            s.dma("pool", [(w1t[0:64], w1v_), (w1t[64:128], w1v_)], writes=[w1b], sembuf=w1b)
            s.dma("pool", [(w2t[:], w2d[kvi])], writes=[w2b], sembuf=w2b)
            s.dma("pool", [(pt_[:], posd[kvi])], writes=[pb_], sembuf=pb_)
            p, pb = rr(ev, pm); ev += 1
            for pp in range(32):
                s.op("pe", lambda e, p=p, pp=pp, w1t=w1t, pt_=pt_: e.matmul(p[:, 0:1], w1t[0:64, pp, :], pt_[0:64, pp:pp + 1],
                                                                       start=(pp == 0), stop=(pp == 31)), reads=[w1b, pb_], writes=[pb])
            s.op("dve", lambda e, p=p, kvi=kvi: e.tensor_copy(out=hb[:, 32 * kvi:32 * kvi + 1], in_=p[:, 0:1]), reads=[pb], writes=[hbb])
            for nch in range(2):
                n0, cnt = nch * 512, (512 if nch == 0 else 511)
                ci, cib = cin[nch]
                ntok = 16 * (cnt - 1) + 32
                s.dma("sp", [(ci[:, 0:ntok], kT[kvi, :, 16 * n0:16 * n0 + ntok])], writes=[cib], sembuf=cib)
                for g in range(2):
                    p, pb = rr(ev, ps); ev += 1
                    for pp in range(32):
                        s.op("pe", lambda e, p=p, pp=pp, g=g, ci=ci, cnt=cnt, w1t=w1t: e.matmul(
                            p[:, 0:cnt], w1t[64 * g:64 * g + 64, pp, :], ci[64 * g:64 * g + 64, pp:pp + 16 * (cnt - 1) + 1:16],
                            start=(pp == 0), stop=(pp == 31)), reads=[w1b, cib], writes=[pb])
                    s.op("act", lambda e, p=p, g=g, n0=n0, cnt=cnt, kvi=kvi: e.activation(
                        out=hid[g][0][:, n0:n0 + cnt], in_=p[:, 0:cnt], func=AF.Silu, bias=hb[:, 32 * kvi:32 * kvi + 1]),
                        reads=[pb, hbb], writes=[hid[g][1]])
            for g in range(2):
                ht, htb = hid[g]
                if kvi == 0:
                    for nch in range(2):
                        p, pb = rr(ev, pm); ev += 1
                        s.op("pe", lambda e, p=p, g=g, nch=nch, ht=ht, w2t=w2t: e.matmul(
                            p[64 * g:64 * g + 64, :], w2t[:, :], ht[:, nch * 512:(nch + 1) * 512], start=True, stop=True),
                            reads=[w2b, htb], writes=[pb])
                        s.op("dve", lambda e, p=p, g=g, nch=nch: e.tensor_copy(out=kc[64 * g:64 * g + 64, nch * 512:(nch + 1) * 512],
                                                                            in_=p[64 * g:64 * g + 64, :]), reads=[pb], writes=[kcb])
                else:
                    for c8 in range(8):
                        p, pb = rr(ev, pm); ev += 1
                        s.op("pe", lambda e, p=p, c8=c8, ht=ht, w2t=w2t: e.matmul(
                            p[:, 0:64], ht[:, c8 * 128:(c8 + 1) * 128], w2t[:, :], start=True, stop=True),
                            reads=[w2b, htb], writes=[pb])
                        s.op("dve", lambda e, p=p, c8=c8, g=g: e.tensor_copy(out=vc[:, c8, g, 0:64], in_=p[:, 0:64]),
                             reads=[pb], writes=[vcb])
        s.dma("sp", [(kco, kc[:])], reads=[kcb], writes=[b_k], sembuf=kcb)
        s.dma("sp", [(vco, vc[:].rearrange("p a g d -> p (a g d)"))], reads=[vcb], writes=[b_v], sembuf=vcb)
        s.finish([b_k, b_v])
        s.emit(st)
    return nc


_PROGS = {}
_DEBUG_LAYERS = 0


def _prog(name, fn):
    if name not in _PROGS:
        _PROGS[name] = fn()
    return _PROGS[name]


def _run(nc, in_maps):
    res = run_bass_kernel_spmd(nc, in_maps, core_ids=list(range(NCORES)))
    return res.results


def own_rows(c):
    return np.concatenate([np.arange(gblock(c, lb) * 128, gblock(c, lb) * 128 + 128) for lb in range(16)])


def kernel(x, ln_attn, w_in, w_out, lam_q1, lam_k1, lam_q2, lam_k2, subln,
           cmp_pos_k, cmp_w1_k, cmp_w2_k, cmp_pos_v, cmp_w1_v, cmp_w2_v,
           ln_ffn, ffn_w_gate, ffn_w_up, ffn_w_down,
           router_w, router_b, exp_w_gate, exp_w_up, exp_w_down, ln_final):
    f32 = lambda a: np.ascontiguousarray(np.asarray(a, dtype=np.float32))
    x = f32(x)[0]
    rows = [own_rows(c) for c in range(NCORES)]
    xs = [np.ascontiguousarray(x[r]) for r in rows]
    idn = ident_np()
    consts = [k2_consts(c) for c in range(NCORES)]
    depth = np.asarray(ln_attn).shape[0]
    xf = None
    for l in range(depth):
        r1 = _run(_prog("k1", build_k1), [{"x": xs[c], "g": f32(ln_attn[l:l + 1]), "w": f32(w_in[l]), "ident": idn} for c in range(NCORES)])
        featT = [np.asarray(r["featT"]) for r in r1]
        kfull = np.empty((12, 128, S), dtype=NPBF)
        vfull = np.empty((S, VCOLS), dtype=NPBF)
        kids = [4, 5, 6, 7, 12, 13, 14, 15, 24, 25, 26, 27]
        for c in range(NCORES):
            kfull[:, :, rows[c]] = featT[c][kids]
            vfull[rows[c]] = np.asarray(r1[c]["tokM"])
        imp = {"kT": np.ascontiguousarray(kfull[8:10]), "w1k": f32(cmp_w1_k[l]), "w1v": f32(cmp_w1_v[l]),
               "w2k": f32(cmp_w2_k[l]), "w2v": f32(cmp_w2_v[l]),
               "poskT": np.ascontiguousarray(f32(cmp_pos_k[l]).T), "posvT": np.ascontiguousarray(f32(cmp_pos_v[l]).T)}
        rp = _run(_prog("k2p", build_k2p), [imp] * NCORES)
        kcT, vcmp = np.asarray(rp[0]["kcT"]), np.asarray(rp[0]["vcmp"])
        lam_init = 0.8 - 0.6 * math.exp(-0.3 * l)
        lamv = np.concatenate([f32(lam_q1[l]), f32(lam_k1[l]), f32(lam_q2[l]), f32(lam_k2[l])])[None]
        lcst = np.array([[lam_init, 1.0 - lam_init]], np.float32)
        kA, vA = np.ascontiguousarray(kfull[0:4]), np.ascontiguousarray(vfull[:, 0:516])
        kB, vB = np.ascontiguousarray(kfull[4:8]), np.ascontiguousarray(vfull[:, 516:1036])
        kC, vC = np.ascontiguousarray(kfull[10:12]), np.ascontiguousarray(vfull[:, 1036:1296])
        ra = _run(_prog("k2a", lambda: build_k2(do_a=True, do_b=False, do_c=False)),
                  [{"kT": kA, "vv": vA, "qT": np.ascontiguousarray(featT[c][0:4]), "cb": consts[c][0], "cf": consts[c][1],
                    "lamv": lamv, "lcst": lcst, "subln": f32(subln[l:l + 1])} for c in range(NCORES)])
        rb = _run(_prog("k2b", lambda: build_k2(do_a=False, do_b=True, do_c=False)),
                  [{"kT": kB, "vv": vB, "qT": np.ascontiguousarray(featT[c][8:12]), "cb": consts[c][0], "cf": consts[c][1]}
                   for c in range(NCORES)])
        rc = _run(_prog("k2c", lambda: build_k2(do_a=False, do_b=False, do_c=True)),
                  [{"kT": kC, "vv": vC, "qT": np.ascontiguousarray(featT[c][16:24]), "gat": np.asarray(r1[c]["gates"]),
                    "cb": consts[c][0], "cf": consts[c][1], "kcT": kcT, "vcmp": vcmp} for c in range(NCORES)])
        mix = [np.ascontiguousarray(np.concatenate([np.asarray(ra[c]["mixo"]), np.asarray(rb[c]["mixo"]), np.asarray(rc[c]["mixo"])], axis=1))
               for c in range(NCORES)]
        j = l // 2
        if l % 2 == 0:
            common = {"wo": f32(w_out[l]), "gf": f32(ln_ffn[l:l + 1]), "ident": idn,
                      "wg": f32(ffn_w_gate[j:j + 1]), "wu": f32(ffn_w_up[j:j + 1]), "wd": f32(ffn_w_down[j:j + 1])}
            r3 = _run(_prog("k3d", lambda: build_k3(1)), [dict(common, x=xs[c], mix=mix[c]) for c in range(NCORES)])
        else:
            common = {"wo": f32(w_out[l]), "gf": f32(ln_ffn[l:l + 1]), "ident": idn,
                      "wg": f32(exp_w_gate[j]), "wu": f32(exp_w_up[j]), "wd": f32(exp_w_down[j]),
                      "wr": f32(router_w[j]), "rb": f32(router_b[j:j + 1]), "gfin": f32(ln_final)[None]}
            r3 = _run(_prog("k3m", lambda: build_k3(8)), [dict(common, x=xs[c], mix=mix[c]) for c in range(NCORES)])
            xf = [np.asarray(r["xf"]) for r in r3]
        xs = [np.asarray(r["xo"]) for r in r3]
        if _DEBUG_LAYERS and l + 1 == _DEBUG_LAYERS:
            return xs, rows
    out = np.empty((S, D), np.float32)
    for c in range(NCORES):
        out[rows[c]] = xf[c]
    return out[None]
```
